# Optimizing a Trainium2 kernel written in Bass

```python
import math
import jax
import jax.numpy as jnp
from jax import lax
import numpy as np

D_MODEL = 1024
BATCH = 8
SEQ = 4096
DEPTH = 1

D_MIX = D_MODEL
D_ATTN = D_MIX // 2
D_SSM = D_MIX - D_ATTN
ATTN_HEADS = 4
ATTN_QK_DIM = D_ATTN // (2 * ATTN_HEADS)
ATTN_V_DIM = D_ATTN // ATTN_HEADS
SSM_GROUP_WIDTH = 16
SSM_GROUPS = D_SSM // SSM_GROUP_WIDTH
SSM_STATE = 64
D_IN_PROJ = 3 * D_ATTN + D_SSM
N_EXPERTS = 32
TOP_K = 4
D_FF = D_MODEL
SWIGLU_LIMIT = 7.0
SWIGLU_ALPHA = 1.702
Q_BLOCK = 128
MOE_BLOCK = 128
RMS_EPS = 1e-6
DT_MIN = 1e-3
DT_MAX = 1e-1
MASK_VALUE = -1e30

kernel_name = 'hybrid_diffattn_s5_moe_block'


def _rmsnorm(x, g):
    xf = x.astype(jnp.float32)
    y = xf * lax.rsqrt(jnp.mean(xf * xf, axis=-1, keepdims=True) + RMS_EPS)
    return (y * g.astype(jnp.float32)).astype(x.dtype)


def _modulate(h, shift, scale):
    return h * (1.0 + scale[:, None, :]) + shift[:, None, :]


def _diff_attention(q, k, v, lam, lambda_init, subln_g):
    b, s, h, _, dk = q.shape
    dv = v.shape[-1]
    n_blk = s // Q_BLOCK
    q_blocks = q.reshape(b, n_blk, Q_BLOCK, h, 2, dk).transpose(1, 0, 2, 3, 4, 5)
    key_pos = jnp.arange(s)
    scale = dk ** -0.5

    def one_block(args):
        q_blk, i = args
        sc = jnp.einsum('bqhmd,bkhmd->bhmqk', q_blk, k).astype(jnp.float32) * scale
        q_pos = i * Q_BLOCK + jnp.arange(Q_BLOCK)
        causal = key_pos[None, :] <= q_pos[:, None]
        p = jax.nn.softmax(jnp.where(causal, sc, MASK_VALUE), axis=-1)
        att = p[:, :, 0] - lam * p[:, :, 1]
        return jnp.einsum('bhqk,bkhe->bqhe', att.astype(v.dtype), v)

    out = lax.map(one_block, (q_blocks, jnp.arange(n_blk)))
    out = out.transpose(1, 0, 2, 3, 4).reshape(b, s, h, dv)
    out = _rmsnorm(out, subln_g) * (1.0 - lambda_init)
    return out.reshape(b, s, h * dv)


def _s5(u, a_re, a_im, log_dt, b_re, b_im, c_re, c_im, d_skip, w_glu, b_glu):
    bsz, s, _ = u.shape
    f32 = jnp.float32
    ug = u.reshape(bsz, s, SSM_GROUPS, SSM_GROUP_WIDTH).astype(f32)
    dt = jnp.exp(log_dt.astype(f32))[:, None]
    lam = lax.complex(jnp.minimum(a_re.astype(f32), -1e-4), a_im.astype(f32))
    lam_bar = jnp.exp(lam * dt)
    b_cplx = lax.complex(b_re.astype(f32), b_im.astype(f32))
    b_bar = ((lam_bar - 1.0) / lam)[..., None] * b_cplx
    bu = jnp.einsum('bsgc,gpc->bsgp', ug.astype(jnp.complex64), b_bar)
    a = jnp.broadcast_to(lam_bar[None, None], (1, s, SSM_GROUPS, SSM_STATE))

    def combine(e_i, e_j):
        a_i, x_i = e_i
        a_j, x_j = e_j
        return a_j * a_i, a_j * x_i + x_j

    _, states = lax.associative_scan(combine, (a, bu), axis=1)
    c_cplx = lax.complex(c_re.astype(f32), c_im.astype(f32))
    y = jnp.einsum('bsgp,gcp->bsgc', states, c_cplx).real
    y = y + d_skip.astype(f32).reshape(SSM_GROUPS, SSM_GROUP_WIDTH) * ug
    y = y.reshape(bsz, s, D_SSM).astype(u.dtype)
    z = jax.nn.gelu(y)
    return z * jax.nn.sigmoid(z @ w_glu + b_glu)


def _moe(h, w_router, b_router, w1, b1, w2, b2):
    bsz, s, d = h.shape
    n_tok = bsz * s
    hf = h.reshape(n_tok, d)
    logits = (hf @ w_router + b_router).astype(jnp.float32)
    top_val, top_idx = lax.top_k(logits, TOP_K)
    gates = jax.nn.softmax(top_val, axis=-1)
    m = n_tok * TOP_K
    flat_e = top_idx.reshape(m)
    flat_tok = jnp.arange(m, dtype=jnp.int32) // TOP_K
    flat_gate = gates.reshape(m)
    order = jnp.argsort(flat_e)
    sorted_e = flat_e[order]
    counts = jnp.bincount(flat_e, length=N_EXPERTS)
    padded = ((counts + MOE_BLOCK - 1) // MOE_BLOCK) * MOE_BLOCK
    pend = jnp.cumsum(padded)
    pstart = pend - padded
    ustart = jnp.cumsum(counts) - counts
    dest = pstart[sorted_e] + (jnp.arange(m) - ustart[sorted_e])
    n_blocks = -(-m // MOE_BLOCK) + N_EXPERTS
    n_rows = n_blocks * MOE_BLOCK
    row_tok = jnp.zeros((n_rows,), jnp.int32).at[dest].set(flat_tok[order])
    row_gate = jnp.zeros((n_rows,), jnp.float32).at[dest].set(flat_gate[order])
    block_e = jnp.minimum(
        jnp.searchsorted(pend, jnp.arange(n_blocks) * MOE_BLOCK, side='right'),
        N_EXPERTS - 1)
    x_blocks = hf[row_tok].reshape(n_blocks, MOE_BLOCK, d)

    def expert_block(args):
        xb, e = args
        gu = xb @ w1[e] + b1[e]
        gate, up = jnp.split(gu, 2, axis=-1)
        gate = jnp.minimum(gate, SWIGLU_LIMIT)
        up = jnp.clip(up, -SWIGLU_LIMIT, SWIGLU_LIMIT)
        glu = gate * jax.nn.sigmoid(SWIGLU_ALPHA * gate)
        return ((up + 1.0) * glu) @ w2[e] + b2[e]

    y_rows = lax.map(expert_block, (x_blocks, block_e)).reshape(n_rows, d)
    y_rows = y_rows * row_gate[:, None].astype(y_rows.dtype)
    y = jnp.zeros((n_tok, d), h.dtype).at[row_tok].add(y_rows.astype(h.dtype))
    return y.reshape(bsz, s, d)


def setup_inputs(seed: int = 0) -> dict:
    key = jax.random.key(seed)
    ks = jax.random.split(key, 32)
    f32 = jnp.float32
    L, D, G, P, W = DEPTH, D_MODEL, SSM_GROUPS, SSM_STATE, SSM_GROUP_WIDTH

    def nrm(k, shape, std):
        return jax.random.normal(k, shape, f32) * std

    a_im_base = jnp.pi * jnp.arange(P, dtype=f32)[None, None, :]
    return {
        'x': nrm(ks[0], (BATCH, SEQ, D), 1.0),
        'c': nrm(ks[1], (BATCH, D), 1.0),
        'w_ada': nrm(ks[2], (L, D, 6 * D), 0.5 * D ** -0.5),
        'b_ada': nrm(ks[3], (L, 6 * D), 0.01),
        'norm1_g': 1.0 + nrm(ks[4], (L, D), 0.01),
        'w_in': nrm(ks[5], (L, D, D_IN_PROJ), D ** -0.5),
        'lq1': nrm(ks[6], (L, ATTN_QK_DIM), 0.1),
        'lk1': nrm(ks[7], (L, ATTN_QK_DIM), 0.1),
        'lq2': nrm(ks[8], (L, ATTN_QK_DIM), 0.1),
        'lk2': nrm(ks[9], (L, ATTN_QK_DIM), 0.1),
        'subln_g': 1.0 + nrm(ks[10], (L, ATTN_V_DIM), 0.01),
        'ssm_a_re': -0.5 + nrm(ks[11], (L, G, P), 0.01),
        'ssm_a_im': a_im_base + nrm(ks[12], (L, G, P), 0.01),
        'ssm_log_dt': jax.random.uniform(ks[13], (L, G), f32,
                                         minval=math.log(DT_MIN), maxval=math.log(DT_MAX)),
        'ssm_b_re': nrm(ks[14], (L, G, P, W), (2 * W) ** -0.5),
        'ssm_b_im': nrm(ks[15], (L, G, P, W), (2 * W) ** -0.5),
        'ssm_c_re': nrm(ks[16], (L, G, W, P), (2 * P) ** -0.5),
        'ssm_c_im': nrm(ks[17], (L, G, W, P), (2 * P) ** -0.5),
        'ssm_d': nrm(ks[18], (L, D_SSM), 1.0),
        'w_glu': nrm(ks[19], (L, D_SSM, D_SSM), D_SSM ** -0.5),
        'b_glu': nrm(ks[20], (L, D_SSM), 0.01),
        'w_out': nrm(ks[21], (L, D_MIX, D), D_MIX ** -0.5),
        'norm2_g': 1.0 + nrm(ks[22], (L, D), 0.01),
        'w_router': nrm(ks[23], (L, D, N_EXPERTS), D ** -0.5),
        'b_router': nrm(ks[24], (L, N_EXPERTS), 0.01),
        'w1': nrm(ks[25], (L, N_EXPERTS, D, 2 * D_FF), D ** -0.5),
        'b1': nrm(ks[26], (L, N_EXPERTS, 2 * D_FF), 0.01),
        'w2': nrm(ks[27], (L, N_EXPERTS, D_FF, D), D_FF ** -0.5),
        'b2': nrm(ks[28], (L, N_EXPERTS, D), 0.01),
        'final_g': 1.0 + nrm(ks[29], (D,), 0.01),
    }


def reference(x, c, w_ada, b_ada, norm1_g, w_in, lq1, lk1, lq2, lk2, subln_g,
              ssm_a_re, ssm_a_im, ssm_log_dt, ssm_b_re, ssm_b_im, ssm_c_re, ssm_c_im,
              ssm_d, w_glu, b_glu, w_out, norm2_g, w_router, b_router, w1, b1, w2, b2,
              final_g):
    bsz, s, _ = x.shape
    c_act = jax.nn.silu(c)
    for l in range(DEPTH):
        lambda_init = 0.8 - 0.6 * math.exp(-0.3 * l)
        mod = c_act @ w_ada[l] + b_ada[l]
        sh1, sc1, g1, sh2, sc2, g2 = jnp.split(mod, 6, axis=-1)

        h = _modulate(_rmsnorm(x, norm1_g[l]), sh1, sc1)
        proj = h @ w_in[l]
        q, k, v, u = jnp.split(proj, [D_ATTN, 2 * D_ATTN, 3 * D_ATTN], axis=-1)
        q = q.reshape(bsz, s, ATTN_HEADS, 2, ATTN_QK_DIM)
        k = k.reshape(bsz, s, ATTN_HEADS, 2, ATTN_QK_DIM)
        v = v.reshape(bsz, s, ATTN_HEADS, ATTN_V_DIM)
        lam = (jnp.exp(jnp.sum(lq1[l].astype(jnp.float32) * lk1[l].astype(jnp.float32)))
               - jnp.exp(jnp.sum(lq2[l].astype(jnp.float32) * lk2[l].astype(jnp.float32)))
               + lambda_init)
        attn_out = _diff_attention(q, k, v, lam, lambda_init, subln_g[l])
        ssm_out = _s5(u, ssm_a_re[l], ssm_a_im[l], ssm_log_dt[l], ssm_b_re[l], ssm_b_im[l],
                      ssm_c_re[l], ssm_c_im[l], ssm_d[l], w_glu[l], b_glu[l])
        mix = jnp.concatenate([attn_out, ssm_out.astype(attn_out.dtype)], axis=-1) @ w_out[l]
        x = x + g1[:, None, :] * mix

        h = _modulate(_rmsnorm(x, norm2_g[l]), sh2, sc2)
        x = x + g2[:, None, :] * _moe(h, w_router[l], b_router[l], w1[l], b1[l], w2[l], b2[l])
    return _rmsnorm(x, final_g)
```

```python
import math
from contextlib import ExitStack

import numpy as np
import concourse.bass as bass
import concourse.mybir as mybir
from concourse.bass_utils import run_bass_kernel_spmd

F32 = mybir.dt.float32
BF16 = mybir.dt.bfloat16
I32 = mybir.dt.int32
AF = mybir.ActivationFunctionType
ALU = mybir.AluOpType

S = 4096
D = 1024
NT = 32
NE = 32
EPS = 1e-6
PI = math.pi
KS = [0, 1, 2, 3, 4, 5, 6, 7, 8, 16, 32, 64, 128, 256, 512, 1024, 2048]
NK = len(KS)
LAMBDA_INIT = 0.8 - 0.6 * math.exp(0.0)


class R:
    __slots__ = ("w", "rs")

    def __init__(self):
        self.w = None
        self.rs = []


def RL(*dims):
    if not dims:
        return R()
    return [RL(*dims[1:]) for _ in range(dims[0])]


def flat(x):
    if isinstance(x, R):
        return [x]
    out = []
    for e in x:
        out.extend(flat(e))
    return out


class Prog:
    ENG = ("pe", "act", "dve", "pool", "sp")
    CE = ("pe", "act", "dve", "pool")

    def __init__(self, nc, stack, n_lanes=32):
        self.nc = nc
        self.stack = stack
        self.seg = 0
        self.sems = {}
        self.sem = {e: self._newsem(e) for e in self.CE}
        self.lanes = [stack.enter_context(nc.semaphore(f"s_dma{i}")) for i in range(n_lanes)]
        self.lane_val = [0] * n_lanes
        self.lane_next = 0
        self.cnt = {e: 0 for e in self.CE}
        self.q = {e: [] for e in self.ENG}
        self.seen = {e: {} for e in self.ENG}

    def _need(self, e, dep, waits, force=False):
        if dep is None:
            return
        key, val = dep
        if isinstance(key[0], str) and key[0] != "L":
            if key[1] < self.seg:
                return
            if key[0] == "pe" and e == "pe" and not force:
                return
        if self.seen[e].get(key, 0) >= val:
            return
        self.seen[e][key] = val
        waits.append((key, val))

    def _deps(self, e, r, w):
        waits = []
        for x in flat(r):
            self._need(e, x.w, waits)
        for x in flat(w):
            self._need(e, x.w, waits)
            for d in x.rs:
                self._need(e, d, waits)
        return waits

    def _commit(self, me, r, w):
        for x in flat(r):
            x.rs.append(me)
        for x in flat(w):
            x.w = me
            x.rs = []

    def op(self, e, fn, r=(), w=()):
        waits = self._deps(e, r, w)
        self.cnt[e] += 1
        me = ((e, self.seg), self.cnt[e])
        self.q[e].append((waits, fn, ((e, self.seg), 1)))
        self._commit(me, r, w)

    def dma(self, fn, r=(), w=(), e="sp"):
        waits = self._deps(e, r, w)
        li = self.lane_next
        self.lane_next = (li + 1) % len(self.lanes)
        if self.lane_val[li] > 0:
            self._need(e, (("L", li), self.lane_val[li]), waits)
        self.lane_val[li] += 16
        me = (("L", li), self.lane_val[li])
        self.q[e].append((waits, fn, (("L", li), 16)))
        self._commit(me, r, w)

    def barrier(self):
        for e in self.ENG:
            waits = []
            for f in self.CE:
                if self.cnt[f] > 0:
                    self._need(e, ((f, self.seg), self.cnt[f]), waits, force=True)
            for li, v in enumerate(self.lane_val):
                if v > 0:
                    self._need(e, (("L", li), v), waits)
            if waits:
                self.q[e].append((waits, None, None))
        self.seg += 1
        for f in self.CE:
            self.cnt[f] = 0
            self._newsem(f)

    def _newsem(self, e):
        k = (e, self.seg)
        self.sems[k] = self.stack.enter_context(self.nc.semaphore(f"s_{e}_{self.seg}"))
        return self.sems[k]

    def _semof(self, key):
        if key[0] == "L":
            return self.lanes[key[1]]
        return self.sems[key]

    def replay(self, block):
        def mk(e):
            def body(engine):
                for waits, fn, inc in self.q[e]:
                    for key, val in waits:
                        engine.wait_ge(self._semof(key), val)
                    if fn is not None:
                        ins = fn(engine)
                        ins.then_inc(self._semof(inc[0]), inc[1])
            return body
        block.tensor(mk("pe"))
        block.scalar(mk("act"))
        block.vector(mk("dve"))
        block.gpsimd(mk("pool"))
        block.sync(mk("sp"))


def build(dbg=False, stop=None, nexp=NE, nqt=4, cut=99):
    nc = bass.Bass("TRN2", target_bir_lowering=False)
    dram = {}

    def din(name, shape):
        dram[name] = nc.dram_tensor(name, list(shape), F32, kind="ExternalInput").ap()
        return dram[name]

    x_d = din("x", [S, D])
    c_d = din("c", [D])
    wada_d = din("w_ada", [D, 6 * D])
    bada_d = din("b_ada", [6 * D])
    n1g_d = din("norm1_g", [D])
    win_d = din("w_in", [D, 2048])
    lqk_d = din("lqk", [4, 64])
    subg_d = din("subln_g", [128])
    are_d = din("ssm_a_re", [32, 64])
    aim_d = din("ssm_a_im", [32, 64])
    ldt_d = din("ssm_log_dt", [32])
    bre_d = din("ssm_b_re", [32, 64, 16])
    bim_d = din("ssm_b_im", [32, 64, 16])
    cre_d = din("ssm_c_re", [32, 16, 64])
    cim_d = din("ssm_c_im", [32, 16, 64])
    sd_d = din("ssm_d", [512])
    wglu_d = din("w_glu", [512, 512])
    bglu_d = din("b_glu", [512])
    wout_d = din("w_out", [D, D])
    n2g_d = din("norm2_g", [D])
    wr_d = din("w_router", [D, NE])
    br_d = din("b_router", [NE])
    full = stop is None or stop >= 9
    w1_d = din("w1", [nexp, D, 2048]) if full else None
    b1_d = din("b1", [NE, 2048])
    w2_d = din("w2", [nexp, D, D]) if full else None
    b2_d = din("b2", [NE, D])
    fg_d = din("final_g", [D])
    out_d = nc.dram_tensor("out", [S, D], F32, kind="ExternalOutput").ap()
    x1s_d = nc.dram_tensor("x1s", [S, D], F32, kind="Internal").ap()
    sso_d = nc.dram_tensor("ssos", [4, 128, S], BF16, kind="Internal").ap()
    dbg_d = {}

    def dout(name, shape, dt=F32):
        dbg_d[name] = nc.dram_tensor(name, list(shape), dt, kind="ExternalOutput").ap()
        return dbg_d[name]

    with ExitStack() as st:
        st.enter_context(nc.allow_non_contiguous_dma(reason="param layouts"))
        st.enter_context(nc.allow_low_precision(reason="bf16 matmul operands, fp32 accumulation"))
        AR = 204
        arena = st.enter_context(nc.sbuf_tensor("arena", [128, AR * 256], F32))
        ps = st.enter_context(nc.psum_tensor("ps", [128, 8, 512], F32))
        P = Prog(nc, st)
        RB = RL(8)

        def finish():
            P.barrier()
            P.dma(lambda e: e.dma_start(out=out_d[0:128, 0:128], in_=arena[:, 0:128]))
            P.barrier()
            with nc.Block() as block:
                P.replay(block)
            return nc

        def A(off_kib, nbytes, dt=F32, parts=128):
            o = int(round(off_kib * 256))
            n32 = (nbytes + 3) // 4
            assert o + n32 <= AR * 256, (off_kib, nbytes)
            v = arena[0:parts, o:o + n32]
            if dt != F32:
                v = v.bitcast(dt)
            return v

        def psb(b, n=512, dt=F32, off=0):
            if dt == F32:
                return ps[:, b, off:off + n]
            return ps[:, b, :].bitcast(dt)[:, off:off + n]

        cb = [0.0]

        def C(nbytes, dt=F32, parts=128):
            v = A(cb[0], nbytes, dt, parts)
            cb[0] += ((nbytes + 63) // 64) * 64 / 1024.0
            return v

        ident_f = C(512)
        J_f = C(512)
        ident_b = C(256, BF16)
        tri_b = C(256, BF16)
        g1_bc = C(4096)
        g2_bc = C(4096)
        fg_bc = C(4096)
        subg_bc = C(512)
        b2t = C(4096)
        b1cols = C(2048)
        b1p1 = C(2048)
        wr_t = C(1024)
        A1c, B1c, A2c, B2c = C(32), C(32), C(32), C(32)
        brt_bc = C(128)
        neglam = C(4)
        bglu_c = C(16)
        sgn = C(4)
        nsgn = C(4)
        epsc = C(4)
        assert cb[0] <= 24.0, cb[0]
        rC = R()

        P.op("pool", lambda e: e.memset(ident_f, 0.0), w=[rC])
        P.op("pool", lambda e: e.affine_select(out=ident_f, in_=ident_f, pattern=[[-1, 128]], compare_op=ALU.not_equal,
                                               fill=1.0, base=0, channel_multiplier=1), r=[rC], w=[rC])
        P.op("pool", lambda e: e.memset(J_f, 0.0), w=[rC])
        P.op("pool", lambda e: e.affine_select(out=J_f[:, 64:128], in_=J_f[:, 64:128], pattern=[[-1, 64]], compare_op=ALU.not_equal,
                                               fill=1.0, base=0, channel_multiplier=1), r=[rC], w=[rC])
        P.op("pool", lambda e: e.affine_select(out=J_f[:, 0:64], in_=J_f[:, 0:64], pattern=[[-1, 64]], compare_op=ALU.not_equal,
                                               fill=1.0, base=-64, channel_multiplier=1), r=[rC], w=[rC])
        P.op("pool", lambda e: e.tensor_copy(out=ident_b, in_=ident_f), r=[rC], w=[rC])
        P.op("pool", lambda e: e.memset(tri_b, 1.0), w=[rC])
        P.op("pool", lambda e: e.affine_select(out=tri_b, in_=tri_b, pattern=[[1, 128]], compare_op=ALU.is_ge,
                                               fill=0.0, base=0, channel_multiplier=-1), r=[rC], w=[rC])
        P.op("pool", lambda e: e.memset(sgn[0:64, :], -1.0), w=[rC])
        P.op("pool", lambda e: e.memset(sgn[64:128, :], 1.0), w=[rC])
        P.op("pool", lambda e: e.memset(nsgn[0:64, :], 1.0), w=[rC])
        P.op("pool", lambda e: e.memset(nsgn[64:128, :], -1.0), w=[rC])
        P.op("pool", lambda e: e.memset(epsc, EPS), w=[rC])
        P.dma(lambda e: e.dma_start(out=fg_bc, in_=fg_d.partition_broadcast(128)), w=[rC])
        P.dma(lambda e: e.dma_start(out=subg_bc[:, 0:128], in_=subg_d.partition_broadcast(128)), w=[rC])
        P.dma(lambda e: e.dma_start(out=b2t[0:32, :], in_=b2_d), w=[rC])
        P.dma(lambda e: e.dma_start(out=wr_t.rearrange("p (k n) -> p k n", k=8), in_=wr_d.rearrange("(k p) n -> p k n", p=128)), w=[rC])
        P.dma(lambda e: e.dma_start(out=brt_bc[:, 0:32], in_=br_d.partition_broadcast(128)), w=[rC])
        P.dma(lambda e: e.dma_start(out=bglu_c[:, 0:4], in_=bglu_d.rearrange("(k p) -> p k", p=128)), w=[rC])
        P.op("dve", lambda e: e.tensor_scalar(out=subg_bc[:, 0:128], in0=subg_bc[:, 0:128], scalar1=1.0 - LAMBDA_INIT, scalar2=None, op0=ALU.mult), r=[rC], w=[rC])

        o0 = 24.0
        mod_bc = A(o0, 24576)
        b_bc = A(o0 + 24, 24576)
        wts = [A(o0 + 48 + 16 * i, 16384).rearrange("p (k n) -> p k n", k=8) for i in range(2)]
        c_col = A(o0 + 80, 32)
        c_rep = A(o0 + 81, 4096).rearrange("p (k n) -> p k n", k=8)
        n1c = A(o0 + 85, 32)
        n2c = A(o0 + 85.5, 32)
        lq = A(o0 + 86, 1024).rearrange("p (a n) -> p a n", a=4)
        lqs = A(o0 + 87, 16)
        junk0 = A(o0 + 88, 512)
        b1s = A(o0 + 89, 2048)
        rwt = RL(2)
        rP0 = R()
        P.dma(lambda e: e.dma_start(out=c_col[:, 0:8], in_=c_d.rearrange("(k p) -> p k", p=128)), w=[rP0])
        P.dma(lambda e: e.dma_start(out=b_bc, in_=bada_d.partition_broadcast(128)), w=[rP0])
        P.dma(lambda e: e.dma_start(out=n1c[:, 0:8], in_=n1g_d.rearrange("(k p) -> p k", p=128)), w=[rP0])
        P.dma(lambda e: e.dma_start(out=n2c[:, 0:8], in_=n2g_d.rearrange("(k p) -> p k", p=128)), w=[rP0])
        P.dma(lambda e: e.dma_start(out=lq, in_=lqk_d.partition_broadcast(128)), w=[rP0])
        P.op("act", lambda e: e.activation(out=c_col[:, 0:8], in_=c_col[:, 0:8], func=AF.Silu), r=[rP0], w=[rP0])
        for k in range(8):
            P.op("dve", lambda e, k=k: e.tensor_copy(out=c_rep[:, k, :], in_=c_col[:, k:k + 1].to_broadcast([128, 128])), r=[rP0], w=[rP0])
        for ns in range(12):
            wt = wts[ns % 2]
            P.dma(lambda e, wt=wt, ns=ns: e.dma_start(out=wt, in_=wada_d[:, ns * 512:(ns + 1) * 512].rearrange("(k p) n -> p k n", p=128)), w=[rwt[ns % 2]])
            b = ns % 2
            for k in range(8):
                P.op("pe", lambda e, wt=wt, k=k, b=b: e.matmul(psb(b), lhsT=c_rep[:, k, :], rhs=wt[:, k, :], start=(k == 0), stop=(k == 7)),
                     r=[rP0, rwt[ns % 2]], w=[RB[b]])
            P.op("dve", lambda e, ns=ns, b=b: e.tensor_tensor(out=mod_bc[:, ns * 512:(ns + 1) * 512], in0=psb(b), in1=b_bc[:, ns * 512:(ns + 1) * 512], op=ALU.add),
                 r=[RB[b], rP0], w=[rP0])
        P.op("dve", lambda e: e.tensor_copy(out=g1_bc, in_=mod_bc[:, 2048:3072]), r=[rP0], w=[rC])
        P.op("dve", lambda e: e.tensor_copy(out=g2_bc, in_=mod_bc[:, 5120:6144]), r=[rP0], w=[rC])

        def diag_col(dst, base):
            for k in range(8):
                P.op("dve", lambda e, k=k: e.scalar_tensor_tensor(out=junk0, in0=mod_bc[:, base + k * 128:base + (k + 1) * 128], scalar=1.0, in1=ident_f,
                                                                   op0=ALU.mult, op1=ALU.mult, accum_out=dst[:, k:k + 1]), r=[rP0, rC], w=[rC])
        diag_col(B1c, 0)
        diag_col(A1c, 1024)
        diag_col(B2c, 3072)
        diag_col(A2c, 4096)
        P.op("dve", lambda e: e.scalar_tensor_tensor(out=A1c[:, 0:8], in0=A1c[:, 0:8], scalar=1.0, in1=n1c[:, 0:8], op0=ALU.add, op1=ALU.mult), r=[rP0, rC], w=[rC])
        P.op("dve", lambda e: e.scalar_tensor_tensor(out=A2c[:, 0:8], in0=A2c[:, 0:8], scalar=1.0, in1=n2c[:, 0:8], op0=ALU.add, op1=ALU.mult), r=[rP0, rC], w=[rC])
        P.op("dve", lambda e: e.scalar_tensor_tensor(out=junk0[:, 0:64], in0=lq[:, 0, :], scalar=1.0, in1=lq[:, 1, :], op0=ALU.mult, op1=ALU.mult, accum_out=lqs[:, 0:1]), r=[rP0], w=[rP0])
        P.op("dve", lambda e: e.scalar_tensor_tensor(out=junk0[:, 0:64], in0=lq[:, 2, :], scalar=1.0, in1=lq[:, 3, :], op0=ALU.mult, op1=ALU.mult, accum_out=lqs[:, 1:2]), r=[rP0], w=[rP0])
        P.op("act", lambda e: e.activation(out=lqs[:, 0:2], in_=lqs[:, 0:2], func=AF.Exp), r=[rP0], w=[rP0])
        P.op("dve", lambda e: e.tensor_tensor(out=lqs[:, 2:3], in0=lqs[:, 1:2], in1=lqs[:, 0:1], op=ALU.subtract), r=[rP0], w=[rP0])
        P.op("dve", lambda e: e.tensor_scalar(out=neglam[:, 0:1], in0=lqs[:, 2:3], scalar1=-LAMBDA_INIT, scalar2=None, op0=ALU.add), r=[rP0], w=[rC])
        b1v = b1_d.rearrange("e (k p) -> (e k) p", p=128)
        for i in range(4):
            P.dma(lambda e, i=i: e.dma_start(out=b1s[:, i * 128:(i + 1) * 128], in_=b1v[i * 128:(i + 1) * 128, :]), w=[rP0])
        for i in range(4):
            P.op("pe", lambda e, i=i: e.transpose(psb(2, 128, off=i * 128), b1s[:, i * 128:(i + 1) * 128], ident_f), r=[rP0, rC], w=[RB[2]])
        P.op("dve", lambda e: e.tensor_copy(out=b1cols[:, 0:512], in_=psb(2)), r=[RB[2]], w=[rC])
        P.op("dve", lambda e: e.tensor_scalar(out=b1p1[:, 0:512], in0=b1cols[:, 0:512], scalar1=1.0, scalar2=None, op0=ALU.add), r=[rC], w=[rC])
        P.barrier()
        if stop == 0:
            return finish()

        def norm_T(src_d, t0, xr, xsr, Acol, Bcol, evac_fn, rXr, rXs, stat, rStat):
            for pr in range(2):
                tiles = []
                for i in (2 * pr, 2 * pr + 1):
                    j = t0 // 128 + i
                    tiles.append((i, j, xr[j % 2], xsr[j % 2], 4 * (j % 4), rStat[j % 4]))
                for i, j, xt, xs, so, rS in tiles:
                    P.dma(lambda e, xt=xt, j=j: e.dma_start(out=xt, in_=src_d[j * 128:(j + 1) * 128, :]), w=[rXr[j % 2]])
                for i, j, xt, xs, so, rS in tiles:
                    P.op("act", lambda e, xt=xt, xs=xs, so=so: e.activation(out=xs, in_=xt, func=AF.Square, accum_out=stat[:, so:so + 1]), r=[rXr[j % 2]], w=[rXs[j % 2], rS])
                for i, j, xt, xs, so, rS in tiles:
                    P.op("dve", lambda e, so=so: e.tensor_scalar(out=stat[:, so + 1:so + 2], in0=stat[:, so:so + 1], scalar1=1.0 / D, scalar2=EPS, op0=ALU.mult, op1=ALU.add), r=[rS], w=[rS])
                for i, j, xt, xs, so, rS in tiles:
                    P.op("act", lambda e, so=so: e.activation(out=stat[:, so + 2:so + 3], in_=stat[:, so + 1:so + 2], func=AF.Sqrt), r=[rS], w=[rS])
                for i, j, xt, xs, so, rS in tiles:
                    P.op("dve", lambda e, so=so: e.reciprocal(out=stat[:, so + 3:so + 4], in_=stat[:, so + 2:so + 3]), r=[rS], w=[rS])
                for i, j, xt, xs, so, rS in tiles:
                    P.op("act", lambda e, xt=xt, xs=xs, so=so: e.activation(out=xs, in_=xt, func=AF.Identity, scale=stat[:, so + 3:so + 4]), r=[rXr[j % 2], rS], w=[rXs[j % 2]])
                for i, j, xt, xs, so, rS in tiles:
                    for kc in range(8):
                        P.op("pe", lambda e, xs=xs, kc=kc, i=i: e.transpose(psb(kc, 128, off=i * 128), xs[:, kc * 128:(kc + 1) * 128], ident_f),
                             r=[rXs[j % 2], rC], w=[RB[kc]])
            for kc in range(8):
                evac_fn(kc)

        Un = [A(24 + 8 * nt, 8192, BF16).rearrange("p (g s c) -> p g s c", g=32, s=8) for nt in range(4)]
        rUn = RL(4)
        wu_st = A(56, 16384).rearrange("p (k n) -> p k n", k=8)
        Wu = A(72, 8192, BF16).rearrange("p (k n) -> p k n", k=8)
        xr = [A(80 + 4 * i, 4096) for i in range(2)]
        xsr = [A(88 + 4 * i, 4096) for i in range(2)]
        h1g = A(96, 16384, BF16).rearrange("p (k n) -> p k n", k=8)
        stat1 = A(112, 64)
        rXr, rXs, rStat, rWu, rH = RL(2), RL(2), RL(4), R(), RL(8)
        P.dma(lambda e: e.dma_start(out=wu_st, in_=win_d[:, 1536:2048].rearrange("(k p) n -> p k n", p=128)), w=[rWu])
        P.op("pool", lambda e: e.tensor_copy(out=Wu, in_=wu_st), r=[rWu], w=[rWu])
        for tg in range(4):
            for half in range(2):
                def ev(kc, half=half):
                    eng = "act" if kc % 2 == 0 else "dve"
                    dst = h1g[:, kc, half * 512:(half + 1) * 512]
                    if eng == "act":
                        P.op("act", lambda e, kc=kc, dst=dst: e.activation(out=dst, in_=psb(kc), func=AF.Identity, scale=A1c[:, kc:kc + 1], bias=B1c[:, kc:kc + 1]),
                             r=[RB[kc], rC], w=[rH[kc]])
                    else:
                        P.op("dve", lambda e, kc=kc, dst=dst: e.tensor_scalar(out=dst, in0=psb(kc), scalar1=A1c[:, kc:kc + 1], scalar2=B1c[:, kc:kc + 1], op0=ALU.mult, op1=ALU.add),
                             r=[RB[kc], rC], w=[rH[kc]])
                norm_T(x_d, tg * 1024 + half * 512, xr, xsr, A1c, B1c, ev, rXr, rXs, stat1, rStat)
            for s in range(8):
                b = s % 2
                for kc in range(8):
                    lhsT = h1g[:, kc, :].rearrange("p (n s) -> p s n", s=8)[:, s, :]
                    P.op("pe", lambda e, lhsT=lhsT, kc=kc, b=b: e.matmul(psb(b), lhsT=lhsT, rhs=Wu[:, kc, :], start=(kc == 0), stop=(kc == 7)),
                         r=[rH[kc], rWu], w=[RB[b]])
                if s % 2 == 0:
                    P.op("act", lambda e, tg=tg, s=s, b=b: e.activation(out=Un[tg][:, :, s, :], in_=psb(b).rearrange("p (g c) -> p g c", g=32), func=AF.Identity), r=[RB[b]], w=[rUn[tg]])
                else:
                    P.op("dve", lambda e, tg=tg, s=s, b=b: e.tensor_copy(out=Un[tg][:, :, s, :], in_=psb(b).rearrange("p (g c) -> p g c", g=32)), r=[RB[b]], w=[rUn[tg]])
        P.barrier()
        if stop == 1:
            return finish()

        Ztok = [A(56 + 8 * nt, 8192, BF16).rearrange("p (t c) -> p t c", t=8) for nt in range(4)]
        Bpow = A(88, 8192, BF16).rearrange("p (g m) -> p g m", g=32)
        Cpw = A(96, 8192, BF16).rearrange("p (g t c) -> p g t c", g=32, t=8)
        Toep = A(104, 8192, BF16).rearrange("p (g m) -> p g m", g=32)
        TB = NK * 32 * 4
        pw_re = A(112, TB).rearrange("p (k g) -> p k g", k=NK)
        pw_im = A(114.25, TB).rearrange("p (k g) -> p k g", k=NK)
        PW2 = A(116.5, TB).rearrange("p (k g) -> p k g", k=NK)
        g0 = 120.0
        T1 = A(g0, TB).rearrange("p (k g) -> p k g", k=NK)
        T2 = A(g0 + 2.25, TB).rearrange("p (k g) -> p k g", k=NK)
        T3 = A(g0 + 4.5, TB).rearrange("p (k g) -> p k g", k=NK)
        T3i = A(g0 + 4.5, TB, I32).rearrange("p (k g) -> p k g", k=NK)
        PBs = A(g0 + 6.75, TB).rearrange("p (k g) -> p k g", k=NK)
        ardt = A(g0 + 9, 128)
        aidt = A(g0 + 9.25, 128)
        dtb = A(g0 + 9.5, 128)
        arr = A(g0 + 9.75, 128)
        aii = A(g0 + 10, 128)
        sm = [A(g0 + 10.25 + 0.125 * i, 128) for i in range(8)]
        bT = [A(g0 + 12 + 2 * i, 2048).rearrange("p (g c) -> p g c", g=32) for i in range(2)]
        cT = [A(g0 + 16 + 2 * i, 2048).rearrange("p (g c) -> p g c", g=32) for i in range(2)]
        Bb = [A(g0 + 20 + 2 * i, 2048).rearrange("p (g c) -> p g c", g=32) for i in range(2)]
        X1 = A(g0 + 24, 2048).rearrange("p (g c) -> p g c", g=32)
        X2 = A(g0 + 26, 2048).rearrange("p (g c) -> p g c", g=32)
        Y1 = A(g0 + 28, 2048).rearrange("p (g c) -> p g c", g=32)
        Y2 = A(g0 + 30, 2048).rearrange("p (g c) -> p g c", g=32)
        tmpA = A(g0 + 32, 2048).rearrange("p (g c) -> p g c", g=32)
        cin = A(g0 + 34, 512)
        BpT = A(g0 + 35, 16384).rearrange("p (g s c) -> p g s c", g=32, s=8)
        CpK = A(g0 + 51, 16384).rearrange("p (g t c) -> p g t c", g=32, t=8)
        Zt = [A(g0 + 67 + i, 960) for i in range(2)]
        dcol = A(g0 + 69, 128)
        rG = R()
        rW = R()

        for h in range(2):
            sl = slice(64 * h, 64 * h + 64)
            P.dma(lambda e, sl=sl: e.dma_start(out=arr[sl, 0:32], in_=are_d.rearrange("g p -> p g")), w=[rG])
            P.dma(lambda e, sl=sl: e.dma_start(out=aii[sl, 0:32], in_=aim_d.rearrange("g p -> p g")), w=[rG])
            P.dma(lambda e, sl=sl: e.dma_start(out=bT[0][sl], in_=bre_d.rearrange("g p c -> p g c")), w=[rG])
            P.dma(lambda e, sl=sl: e.dma_start(out=bT[1][sl], in_=bim_d.rearrange("g p c -> p g c")), w=[rG])
        P.dma(lambda e: e.dma_start(out=dtb[:, 0:32], in_=ldt_d.partition_broadcast(128)), w=[rG])
        for s in range(8):
            P.dma(lambda e, s=s: e.dma_start(out=dcol[16 * s:16 * s + 16, 0:32], in_=sd_d.rearrange("(g c) -> c g", c=16)), w=[rG])
        for ci, cd in enumerate((cre_d, cim_d)):
            cv = cd.rearrange("g c p -> (g c) p")
            for i in range(4):
                for h in range(2):
                    P.dma(lambda e, i=i, h=h, cv=cv: e.dma_start(out=cin[:, 64 * h:64 * h + 64], in_=cv[i * 128:(i + 1) * 128, :]), w=[rG])
                P.op("pe", lambda e, i=i: e.transpose(psb(3, 128, off=i * 128), cin[:, 0:128], ident_f), r=[rG, rC], w=[RB[3]])
            P.op("dve", lambda e, ci=ci: e.tensor_copy(out=cT[ci].rearrange("p g c -> p (g c)"), in_=psb(3)), r=[RB[3]], w=[rG])

        P.op("act", lambda e: e.activation(out=dtb[:, 0:32], in_=dtb[:, 0:32], func=AF.Exp), r=[rG], w=[rG])
        P.op("dve", lambda e: e.tensor_scalar(out=arr[:, 0:32], in0=arr[:, 0:32], scalar1=-1e-4, scalar2=None, op0=ALU.min), r=[rG], w=[rG])
        P.op("dve", lambda e: e.tensor_tensor(out=ardt[:, 0:32], in0=arr[:, 0:32], in1=dtb[:, 0:32], op=ALU.mult), r=[rG], w=[rG])
        P.op("dve", lambda e: e.tensor_tensor(out=aidt[:, 0:32], in0=aii[:, 0:32], in1=dtb[:, 0:32], op=ALU.mult), r=[rG], w=[rG])
        for ki, k in enumerate(KS):
            P.op("act", lambda e, ki=ki, k=k: e.activation(out=T1[:, ki, :], in_=ardt[:, 0:32], func=AF.Exp, scale=float(k)), r=[rG], w=[rG])
            P.op("dve", lambda e, ki=ki, k=k: e.tensor_scalar(out=T2[:, ki, :], in0=aidt[:, 0:32], scalar1=float(k), scalar2=None, op0=ALU.mult), r=[rG], w=[rG])
        fl = lambda t: t.rearrange("p k g -> p (k g)")

        def reduce_sin(dst, src):
            P.op("dve", lambda e: e.tensor_scalar(out=fl(T3), in0=fl(src), scalar1=1.0 / (2 * PI), scalar2=None, op0=ALU.mult), r=[rG], w=[rG])
            P.op("dve", lambda e: e.tensor_copy(out=fl(T3i), in_=fl(T3)), r=[rG], w=[rG])
            P.op("dve", lambda e: e.tensor_copy(out=fl(T3), in_=fl(T3i)), r=[rG], w=[rG])
            P.op("dve", lambda e: e.scalar_tensor_tensor(out=fl(T3), in0=fl(T3), scalar=-2 * PI, in1=fl(src), op0=ALU.mult, op1=ALU.add), r=[rG], w=[rG])
            P.op("dve", lambda e: e.tensor_scalar(out=fl(T3), in0=fl(T3), scalar1=PI, scalar2=-PI, op0=ALU.min, op1=ALU.max), r=[rG], w=[rG])
            P.op("act", lambda e: e.activation(out=fl(dst), in_=fl(T3), func=AF.Sin), r=[rG], w=[rG])
        reduce_sin(pw_im, T2)
        P.op("dve", lambda e: e.tensor_scalar(out=fl(T2), in0=fl(T2), scalar1=PI / 2, scalar2=None, op0=ALU.add), r=[rG], w=[rG])
        reduce_sin(pw_re, T2)
        P.op("dve", lambda e: e.tensor_tensor(out=fl(pw_re), in0=fl(pw_re), in1=fl(T1), op=ALU.mult), r=[rG], w=[rG])
        P.op("dve", lambda e: e.tensor_tensor(out=fl(pw_im), in0=fl(pw_im), in1=fl(T1), op=ALU.mult), r=[rG], w=[rG])
        P.op("dve", lambda e: e.tensor_scalar(out=fl(PBs), in0=fl(pw_im), scalar1=sgn[:, 0:1], scalar2=None, op0=ALU.mult), r=[rG, rC], w=[rG])
        P.op("dve", lambda e: e.tensor_scalar(out=fl(PW2), in0=fl(pw_im), scalar1=nsgn[:, 0:1], scalar2=None, op0=ALU.mult), r=[rG, rC], w=[rG])
        nr, ni, den, t0_, t1_, cre_, cim_, t2_ = [s_[:, 0:32] for s_ in sm]
        P.op("dve", lambda e: e.tensor_scalar(out=nr, in0=pw_re[:, 1, :], scalar1=-1.0, scalar2=None, op0=ALU.add), r=[rG], w=[rG])
        P.op("dve", lambda e: e.tensor_copy(out=ni, in_=pw_im[:, 1, :]), r=[rG], w=[rG])
        P.op("dve", lambda e: e.tensor_tensor(out=den, in0=arr[:, 0:32], in1=arr[:, 0:32], op=ALU.mult), r=[rG], w=[rG])
        P.op("dve", lambda e: e.tensor_tensor(out=t0_, in0=aii[:, 0:32], in1=aii[:, 0:32], op=ALU.mult), r=[rG], w=[rG])
        P.op("dve", lambda e: e.tensor_tensor(out=den, in0=den, in1=t0_, op=ALU.add), r=[rG], w=[rG])
        P.op("dve", lambda e: e.reciprocal(out=den, in_=den), r=[rG], w=[rG])
        P.op("dve", lambda e: e.tensor_tensor(out=t0_, in0=nr, in1=arr[:, 0:32], op=ALU.mult), r=[rG], w=[rG])
        P.op("dve", lambda e: e.tensor_tensor(out=t1_, in0=ni, in1=aii[:, 0:32], op=ALU.mult), r=[rG], w=[rG])
        P.op("dve", lambda e: e.tensor_tensor(out=t0_, in0=t0_, in1=t1_, op=ALU.add), r=[rG], w=[rG])
        P.op("dve", lambda e: e.tensor_tensor(out=cre_, in0=t0_, in1=den, op=ALU.mult), r=[rG], w=[rG])
        P.op("dve", lambda e: e.tensor_tensor(out=t0_, in0=ni, in1=arr[:, 0:32], op=ALU.mult), r=[rG], w=[rG])
        P.op("dve", lambda e: e.tensor_tensor(out=t1_, in0=nr, in1=aii[:, 0:32], op=ALU.mult), r=[rG], w=[rG])
        P.op("dve", lambda e: e.tensor_tensor(out=t0_, in0=t0_, in1=t1_, op=ALU.subtract), r=[rG], w=[rG])
        P.op("dve", lambda e: e.tensor_tensor(out=cim_, in0=t0_, in1=den, op=ALU.mult), r=[rG], w=[rG])
        bc16 = lambda v: v.rearrange("p (g o) -> p g o", o=1).to_broadcast([128, 32, 16])
        P.op("dve", lambda e: e.tensor_tensor(out=Bb[0], in0=bT[0], in1=bc16(cre_), op=ALU.mult), r=[rG], w=[rG])
        P.op("dve", lambda e: e.tensor_tensor(out=tmpA, in0=bT[1], in1=bc16(cim_), op=ALU.mult), r=[rG], w=[rG])
        P.op("dve", lambda e: e.tensor_tensor(out=Bb[0], in0=Bb[0], in1=tmpA, op=ALU.subtract), r=[rG], w=[rG])
        P.op("dve", lambda e: e.tensor_tensor(out=Bb[1], in0=bT[1], in1=bc16(cre_), op=ALU.mult), r=[rG], w=[rG])
        P.op("dve", lambda e: e.tensor_tensor(out=tmpA, in0=bT[0], in1=bc16(cim_), op=ALU.mult), r=[rG], w=[rG])
        P.op("dve", lambda e: e.tensor_tensor(out=Bb[1], in0=Bb[1], in1=tmpA, op=ALU.add), r=[rG], w=[rG])
        lo, hi = slice(0, 64), slice(64, 128)
        P.op("dve", lambda e: e.tensor_copy(out=X1[lo], in_=Bb[0][lo]), r=[rG], w=[rG])
        P.op("dve", lambda e: e.tensor_copy(out=X1[hi], in_=Bb[1][hi]), r=[rG], w=[rG])
        P.op("dve", lambda e: e.tensor_copy(out=X2[lo], in_=Bb[1][lo]), r=[rG], w=[rG])
        P.op("dve", lambda e: e.tensor_copy(out=X2[hi], in_=Bb[0][hi]), r=[rG], w=[rG])
        P.op("dve", lambda e: e.tensor_copy(out=Y1[lo], in_=cT[0][lo]), r=[rG], w=[rG])
        P.op("dve", lambda e: e.tensor_scalar(out=Y1[hi], in0=cT[1][hi], scalar1=-1.0, scalar2=None, op0=ALU.mult), r=[rG], w=[rG])
        P.op("dve", lambda e: e.tensor_copy(out=Y2[lo], in_=cT[1][lo]), r=[rG], w=[rG])
        P.op("dve", lambda e: e.tensor_copy(out=Y2[hi], in_=cT[0][hi]), r=[rG], w=[rG])
        for s in range(8):
            ki = 7 - s
            P.op("dve", lambda e, ki=ki: e.tensor_tensor(out=tmpA, in0=X2, in1=bc16(PBs[:, ki, :]), op=ALU.mult), r=[rG], w=[rG])
            P.op("dve", lambda e, ki=ki, s=s: e.tensor_tensor(out=BpT[:, :, s, :], in0=X1, in1=bc16(pw_re[:, ki, :]), op=ALU.mult), r=[rG], w=[rG])
            P.op("dve", lambda e, s=s: e.tensor_tensor(out=BpT[:, :, s, :], in0=BpT[:, :, s, :], in1=tmpA, op=ALU.add), r=[rG], w=[rG])
        for t in range(8):
            P.op("dve", lambda e, t=t: e.tensor_tensor(out=tmpA, in0=Y2, in1=bc16(pw_im[:, t, :]), op=ALU.mult), r=[rG], w=[rG])
            P.op("dve", lambda e, t=t: e.tensor_tensor(out=CpK[:, :, t, :], in0=Y1, in1=bc16(pw_re[:, t, :]), op=ALU.mult), r=[rG], w=[rG])
            P.op("dve", lambda e, t=t: e.tensor_tensor(out=CpK[:, :, t, :], in0=CpK[:, :, t, :], in1=tmpA, op=ALU.subtract), r=[rG], w=[rG])
        for t in range(8):
            P.op("dve", lambda e, t=t: e.tensor_tensor(out=tmpA, in0=Y2, in1=bc16(pw_im[:, t + 1, :]), op=ALU.mult), r=[rG], w=[rG])
            P.op("dve", lambda e, t=t: e.tensor_tensor(out=Cpw[:, :, t, :], in0=Y1, in1=bc16(pw_re[:, t + 1, :]), op=ALU.mult), r=[rG], w=[rW])
            P.op("dve", lambda e, t=t: e.tensor_tensor(out=Cpw[:, :, t, :], in0=Cpw[:, :, t, :], in1=tmpA, op=ALU.subtract), r=[rG, rW], w=[rW])
        rZ = RL(2)
        for i in range(2):
            P.op("pool", lambda e, i=i: e.memset(Zt[i][:, 0:240], 0.0), w=[rZ[i]])
        for g in range(32):
            b = 4 + g % 2
            P.op("pe", lambda e, g=g, b=b: e.transpose(psb(b, 128), BpT[:, g, :, :].rearrange("p s c -> p (s c)"), ident_f), r=[rG, rC], w=[RB[b]])
            P.op("act", lambda e, g=g, b=b: e.activation(out=Bpow[:, g, :], in_=psb(b, 128), func=AF.Identity), r=[RB[b]], w=[rW])
            z = Zt[g % 2]
            P.op("dve", lambda e, g=g, z=z: e.tensor_copy(out=z[:, 112:128], in_=X1[:, g, :]), r=[rG], w=[rZ[g % 2]])
            b2 = 6 + g % 2
            for s in range(8):
                P.op("pe", lambda e, g=g, s=s, z=z, b2=b2: e.matmul(psb(b2, 128 - 16 * s, off=16 * s), lhsT=z[:, (7 - s) * 16:(7 - s) * 16 + 128],
                                                                     rhs=CpK[:, g, 0:8 - s, :].rearrange("p t c -> p (t c)"), start=(s == 0), stop=(s == 7), skip_group_check=True),
                     r=[rZ[g % 2], rG], w=[RB[b2]])
            P.op("dve", lambda e, g=g, b2=b2: e.scalar_tensor_tensor(out=Toep[:, g, :], in0=ident_f, scalar=dcol[:, g:g + 1], in1=psb(b2, 128), op0=ALU.mult, op1=ALU.add),
                 r=[RB[b2], rG, rC], w=[rW])
        P.barrier()
        if stop == 2:
            return finish()

        m0 = 120.0
        Dk = A(m0, 18432, BF16).rearrange("p (g j m) -> p g j m", g=8, j=9)
        Ug = [A(m0 + 18 + i, 1024, BF16) for i in range(2)]
        Xs = [A(m0 + 20 + i, 1024, BF16) for i in range(4)]
        gw = [[A(m0 + 24 + 6 * i + 2 * k_, 2048) for k_ in range(3)] for i in range(2)]
        zg = [A(m0 + 36 + i, 1024, BF16) for i in range(2)]
        Tt = A(m0 + 38, 512)
        rDk, rUg, rX, rGw, rZg, rZtok, rTt = RL(8), RL(2), RL(4), RL(2), RL(2), RL(4), R()
        for gb in range(4):
            for gl in range(8):
                g = gb * 8 + gl
                for j in range(9):
                    ki = 8 + j
                    P.op("act", lambda e, g=g, ki=ki: e.activation(out=Tt[:, 0:128], in_=ident_f, func=AF.Identity, scale=pw_re[:, ki, g:g + 1]), r=[rW, rC], w=[rTt])
                    P.op("dve", lambda e, g=g, gl=gl, j=j, ki=ki: e.scalar_tensor_tensor(out=Dk[:, gl, j, :], in0=J_f, scalar=PW2[:, ki, g:g + 1], in1=Tt[:, 0:128], op0=ALU.mult, op1=ALU.add),
                         r=[rTt, rW, rC], w=[rDk[gl]])
            for gl in range(8):
                g = gb * 8 + gl
                if cut < 1:
                    continue
                ug = Ug[g % 2]
                pb = g % 2
                for nt in range(4):
                    P.op("pe", lambda e, g=g, nt=nt, pb=pb: e.transpose(psb(pb, 128, BF16, off=nt * 128), Un[nt][:, g, :, :].rearrange("p s c -> p (s c)"), ident_b), r=[rUn[nt], rC], w=[RB[pb]])
                P.op("act", lambda e, ug=ug, pb=pb: e.activation(out=ug, in_=psb(pb, 512, BF16), func=AF.Identity), r=[RB[pb]], w=[rUg[g % 2]])
                kb = 2 + g % 2
                xa = (g % 2) * 2
                P.op("pe", lambda e, g=g, ug=ug, kb=kb: e.matmul(psb(kb), lhsT=Bpow[:, g, :], rhs=ug, start=True, stop=True), r=[rW, rUg[g % 2]], w=[RB[kb]])
                P.op("dve", lambda e, kb=kb, xa=xa: e.tensor_copy(out=Xs[xa], in_=psb(kb)), r=[RB[kb]], w=[rX[xa]])
                cur = xa
                if cut < 2:
                    continue
                for j in range(9 if cut >= 3 else 0):
                    sh = 1 << j
                    nxt = xa + (1 - (cur - xa))
                    P.op("pe", lambda e, cur=cur, kb=kb: e.matmul(psb(kb), lhsT=ident_b, rhs=Xs[cur], start=True, stop=False, skip_group_check=True), r=[rX[cur], rC], w=[RB[kb]])
                    P.op("pe", lambda e, cur=cur, kb=kb, sh=sh, gl=gl, j=j: e.matmul(psb(kb, 512 - sh, off=sh), lhsT=Dk[:, gl, j, :], rhs=Xs[cur][:, 0:512 - sh], start=False, stop=True, skip_group_check=True),
                         r=[rX[cur], rDk[gl]], w=[RB[kb]])
                    if j % 2 == 0:
                        P.op("act", lambda e, nxt=nxt, kb=kb: e.activation(out=Xs[nxt], in_=psb(kb), func=AF.Identity), r=[RB[kb]], w=[rX[nxt]])
                    else:
                        P.op("dve", lambda e, nxt=nxt, kb=kb: e.tensor_copy(out=Xs[nxt], in_=psb(kb)), r=[RB[kb]], w=[rX[nxt]])
                    cur = nxt
                if cut < 4:
                    continue
                P.op("pe", lambda e, g=g, ug=ug, pb=pb: e.matmul(psb(pb), lhsT=Toep[:, g, :], rhs=ug, start=True, stop=False, skip_group_check=True), r=[rW, rUg[g % 2]], w=[RB[pb]])
                P.op("pe", lambda e, g=g, cur=cur, pb=pb: e.matmul(psb(pb, 511, off=1), lhsT=Cpw[:, g, :, :].rearrange("p t c -> p (t c)"), rhs=Xs[cur][:, 0:511], start=False, stop=True, skip_group_check=True),
                     r=[rW, rX[cur]], w=[RB[pb]])
                if cut < 5:
                    continue
                w0, w1_, w2_ = gw[g % 2]
                rg = rGw[g % 2]
                P.op("act", lambda e, w0=w0, pb=pb: e.activation(out=w0[:, 0:512], in_=psb(pb), func=AF.Square), r=[RB[pb]], w=[rg])
                P.op("dve", lambda e, w0=w0: e.tensor_scalar(out=w0[:, 0:512], in0=w0[:, 0:512], scalar1=0.044715, scalar2=1.0, op0=ALU.mult, op1=ALU.add), r=[rg], w=[rg])
                P.op("dve", lambda e, w0=w0, w1_=w1_, pb=pb: e.tensor_tensor(out=w1_[:, 0:512], in0=w0[:, 0:512], in1=psb(pb), op=ALU.mult), r=[rg, RB[pb]], w=[rg])
                P.op("act", lambda e, w1_=w1_: e.activation(out=w1_[:, 0:512], in_=w1_[:, 0:512], func=AF.Sigmoid, scale=1.5957691216), r=[rg], w=[rg])
                zz = zg[g % 2]
                P.op("dve", lambda e, w1_=w1_, zz=zz, pb=pb: e.tensor_tensor(out=zz, in0=w1_[:, 0:512], in1=psb(pb), op=ALU.mult), r=[rg, RB[pb]], w=[rZg[g % 2]])
                if cut < 6:
                    continue
                tb = 4 + g % 2
                for nt in range(4):
                    P.op("pe", lambda e, zz=zz, nt=nt, tb=tb: e.transpose(psb(tb, 128, BF16, off=nt * 128), zz[:, nt * 128:(nt + 1) * 128], ident_b), r=[rZg[g % 2], rC], w=[RB[tb]])
                for nt in range(4):
                    src = psb(tb, 128, BF16, off=nt * 128).rearrange("p (t c) -> p t c", t=8)
                    eng = "act"
                    if eng == "act":
                        P.op("act", lambda e, src=src, nt=nt, g=g: e.activation(out=Ztok[nt][:, :, 16 * g:16 * g + 16], in_=src, func=AF.Identity), r=[RB[tb]], w=[rZtok[nt]])
                    else:
                        P.op("dve", lambda e, src=src, nt=nt, g=g: e.tensor_copy(out=Ztok[nt][:, :, 16 * g:16 * g + 16], in_=src), r=[RB[tb]], w=[rZtok[nt]])
        P.barrier()
        if stop == 3:
            return finish()
        zT = [A(24 + 8 * cc, 8192, BF16) for cc in range(4)]
        rzT = RL(4)
        k_ = 0
        for nt in range(4):
            for cc in range(4):
                b = k_ % 4
                k_ += 1
                for t in range(8):
                    P.op("pe", lambda e, nt=nt, cc=cc, t=t, b=b: e.transpose(psb(b, 128, BF16, off=t * 128), Ztok[nt][:, t, cc * 128:(cc + 1) * 128], ident_b), r=[rZtok[nt], rC], w=[RB[b]])
                dst = zT[cc][:, nt * 1024:(nt + 1) * 1024].rearrange("p (n t) -> p t n", t=8)
                src = psb(b, 1024, BF16).rearrange("p (t n) -> p t n", t=8)
                P.op("act", lambda e, dst=dst, src=src: e.activation(out=dst, in_=src, func=AF.Identity), r=[RB[b]], w=[rzT[cc]])
        P.barrier()
        if stop == 4:
            return finish()
        soT = [A(56 + 8 * cc, 8192, BF16) for cc in range(4)]
        wg_st = A(120, 8192).rearrange("p (k n) -> p k n", k=4)
        Wg = A(128, 4096, BF16).rearrange("p (k n) -> p k n", k=4)
        sgw = [A(132 + i, 1024, BF16) for i in range(2)]
        rWg, rSg, rSo = R(), RL(2), RL(4)
        P.dma(lambda e: e.dma_start(out=wg_st, in_=wglu_d.rearrange("(k p) n -> p k n", p=128)), w=[rWg])
        P.op("pool", lambda e: e.tensor_copy(out=Wg, in_=wg_st), r=[rWg], w=[rWg])
        k_ = 0
        for sl in range(8):
            for co in range(4):
                b = k_ % 4
                sg_ = sgw[k_ % 2]
                rs_ = rSg[k_ % 2]
                k_ += 1
                for ci in range(4):
                    P.op("pe", lambda e, co=co, ci=ci, sl=sl, b=b: e.matmul(psb(b), lhsT=Wg[:, ci, co * 128:(co + 1) * 128], rhs=zT[ci][:, sl * 512:(sl + 1) * 512], start=(ci == 0), stop=(ci == 3)),
                         r=[rWg, rzT[ci]], w=[RB[b]])
                P.op("act", lambda e, co=co, b=b, sg_=sg_: e.activation(out=sg_, in_=psb(b), func=AF.Sigmoid, bias=bglu_c[:, co:co + 1]), r=[RB[b], rC], w=[rs_])
                P.op("dve", lambda e, co=co, sl=sl, sg_=sg_: e.tensor_tensor(out=soT[co][:, sl * 512:(sl + 1) * 512], in0=zT[co][:, sl * 512:(sl + 1) * 512], in1=sg_, op=ALU.mult),
                     r=[rs_, rzT[co]], w=[rSo[co]])
        rSSO = R()
        for cc in range(4):
            P.dma(lambda e, cc=cc: e.dma_start(out=sso_d[cc], in_=soT[cc]), r=[rSo[cc]], w=[rSSO])
        if dbg:
            dz = dout("dbg_so", [4, 128, S], BF16)
            for cc in range(4):
                P.dma(lambda e, cc=cc: e.dma_start(out=dz[cc], in_=soT[cc]), r=[rSo[cc]])
        P.barrier()
        if stop == 5:
            return finish()

        QT = [A(24 + 8 * h, 8192, BF16) for h in range(4)]
        KT = [A(56 + 8 * h, 8192, BF16) for h in range(4)]
        VA = A(88, 33024, BF16).rearrange("p (j h c) -> p j h c", j=32, h=4)
        Wq = A(122, 24576, BF16).rearrange("p (k n) -> p k n", k=8)
        wq_st = [A(146 + 6 * i, 6144) for i in range(2)]
        xr = [A(158 + 4 * i, 4096) for i in range(2)]
        xsr = [A(166 + 4 * i, 4096) for i in range(2)]
        h1g = A(174, 8192, BF16).rearrange("p (k n) -> p k n", k=8)
        stat1 = A(182, 64)
        rXr, rXs, rStat, rWq, rWst, rH = RL(2), RL(2), RL(4), R(), RL(2), RL(8)
        rQ, rK, rV = RL(4), RL(4), RL(32)
        for k in range(8):
            P.dma(lambda e, k=k: e.dma_start(out=wq_st[k % 2][:, 0:1536], in_=win_d[k * 128:(k + 1) * 128, 0:1536]), w=[rWst[k % 2]])
            P.op("pool", lambda e, k=k: e.tensor_copy(out=Wq[:, k, :], in_=wq_st[k % 2][:, 0:1536]), r=[rWst[k % 2]], w=[rWq])
        P.op("pool", lambda e: e.memset(VA[:, :, :, 128:129], 1.0), w=[rV])
        for tg in range(8):
            def ev(kc):
                dst = h1g[:, kc, :]
                if kc % 2 == 0:
                    P.op("act", lambda e, kc=kc, dst=dst: e.activation(out=dst, in_=psb(kc), func=AF.Identity, scale=A1c[:, kc:kc + 1], bias=B1c[:, kc:kc + 1]),
                         r=[RB[kc], rC], w=[rH[kc]])
                else:
                    P.op("dve", lambda e, kc=kc, dst=dst: e.tensor_scalar(out=dst, in0=psb(kc), scalar1=A1c[:, kc:kc + 1], scalar2=B1c[:, kc:kc + 1], op0=ALU.mult, op1=ALU.add),
                         r=[RB[kc], rC], w=[rH[kc]])
            norm_T(x_d, tg * 512, xr, xsr, A1c, B1c, ev, rXr, rXs, stat1, rStat)
            k_ = 0
            for h in range(4):
                for which in range(2):
                    b = k_ % 4
                    k_ += 1
                    col0 = which * 512 + h * 128
                    for kc in range(8):
                        P.op("pe", lambda e, kc=kc, b=b, col0=col0: e.matmul(psb(b), lhsT=Wq[:, kc, col0:col0 + 128], rhs=h1g[:, kc, :], start=(kc == 0), stop=(kc == 7)),
                             r=[rWq, rH[kc]], w=[RB[b]])
                    if which == 0:
                        P.op("act", lambda e, h=h, b=b, tg=tg: e.activation(out=QT[h][:, tg * 512:(tg + 1) * 512], in_=psb(b), func=AF.Identity, scale=0.125), r=[RB[b]], w=[rQ[h]])
                    else:
                        P.op("dve", lambda e, h=h, b=b, tg=tg: e.tensor_copy(out=KT[h][:, tg * 512:(tg + 1) * 512], in_=psb(b)), r=[RB[b]], w=[rK[h]])
            for i in range(4):
                j = tg * 4 + i
                b = 4 + i % 4
                for kc in range(8):
                    P.op("pe", lambda e, kc=kc, b=b, i=i: e.matmul(psb(b), lhsT=h1g[:, kc, i * 128:(i + 1) * 128], rhs=Wq[:, kc, 1024:1536], start=(kc == 0), stop=(kc == 7)),
                         r=[rWq, rH[kc]], w=[RB[b]])
                src = psb(b).rearrange("p (h c) -> p h c", h=4)
                if i % 2 == 0:
                    P.op("act", lambda e, j=j, src=src: e.activation(out=VA[:, j, :, 0:128], in_=src, func=AF.Identity), r=[RB[b]], w=[rV[j]])
                else:
                    P.op("dve", lambda e, j=j, src=src: e.tensor_copy(out=VA[:, j, :, 0:128], in_=src), r=[RB[b]], w=[rV[j]])
        P.barrier()
        if stop == 6:
            return finish()
        if dbg:
            dq = dout("dbg_qT", [128, S], BF16)
            dk_ = dout("dbg_kT", [128, S], BF16)
            dv = dout("dbg_v", [128, 516], BF16)
            P.dma(lambda e: e.dma_start(out=dq, in_=QT[1]))
            P.dma(lambda e: e.dma_start(out=dk_, in_=KT[1]))
            P.dma(lambda e: e.dma_start(out=dv, in_=VA[:, 3, :, :].rearrange("p h c -> p (h c)")))
            P.barrier()

        Wo = A(122, 16384, BF16).rearrange("p (k n) -> p k n", k=8)
        wo_st = [A(138 + 4 * i, 4096) for i in range(2)]
        PT = [[A(146 + 2 * m + i, 1024, BF16) for i in range(2)] for m in range(2)]
        ew = [[A(150 + i + 0.5 * k_, 512) for k_ in range(2)] for i in range(2)]
        atok = [A(154 + j, 1024, BF16) for j in range(4)]
        catT = A(158, 1024, BF16).rearrange("p (c n) -> p c n", c=4)
        ssl = [A(162 + 4 * i, 4096, BF16).rearrange("p (c n) -> p c n", c=4) for i in range(2)]
        xr = [A(170 + 4 * i, 4096) for i in range(2)]
        x1o = [A(178 + 4 * i, 4096) for i in range(2)]
        est = A(186, 64)
        rWo, rWos, rPT, rEw, rAt, rCat, rSsl, rXr, rX1, rEst = R(), RL(2), RL(2, 2), RL(2), RL(4), R(), RL(2), RL(2), RL(2), R()
        for k in range(8):
            P.dma(lambda e, k=k: e.dma_start(out=wo_st[k % 2], in_=wout_d[k * 128:(k + 1) * 128, :]), w=[rWos[k % 2]])
            P.op("pool", lambda e, k=k: e.tensor_copy(out=Wo[:, k, :], in_=wo_st[k % 2]), r=[rWos[k % 2]], w=[rWo])
        def oacc(m, j):
            idx = m * 4 + j
            return ps[:, 4 + idx // 3, (idx % 3) * 129:(idx % 3) * 129 + 129], 4 + idx // 3, (idx % 3 == 0)
        ecount = [0]
        steps = [(g, h, kb) for g in range(8) for h in range(4) for kb in range(4 * g + 4)]

        def emit_S(i):
            g, h, kb = steps[i]
            buf = i % 2
            if h == 0 and kb == 0:
                P.dma(lambda e, g=g: e.dma_start(out=ssl[g % 2], in_=sso_d[:, :, g * 512:(g + 1) * 512].rearrange("c p n -> p c n")), r=[rSSO], w=[rSsl[g % 2]])
            c0 = max(0, kb - 4 * g) * 128
            for m in range(2):
                sb = m * 2 + buf
                P.op("pe", lambda e, m=m, sb=sb, h=h, kb=kb, g=g, c0=c0: e.matmul(psb(sb, 512 - c0, off=c0), lhsT=KT[h][64 * m:64 * m + 64, kb * 128:(kb + 1) * 128],
                                                                                 rhs=QT[h][64 * m:64 * m + 64, g * 512 + c0:(g + 1) * 512], start=True, stop=True),
                     r=[rK[h], rQ[h]], w=[RB[sb]])

        def emit_PV(i):
            g, h, kb = steps[i]
            buf = i % 2
            j0 = max(0, kb - 4 * g)
            c0 = j0 * 128
            for m in range(2):
                sb = m * 2 + buf
                P.op("act", lambda e, m=m, sb=sb, buf=buf, c0=c0: e.activation(out=PT[m][buf][:, c0:512], in_=psb(sb, 512 - c0, off=c0), func=AF.Exp), r=[RB[sb]], w=[rPT[m][buf]])
                if kb >= 4 * g:
                    P.op("pool", lambda e, m=m, buf=buf, c0=c0: e.tensor_tensor(out=PT[m][buf][:, c0:c0 + 128], in0=PT[m][buf][:, c0:c0 + 128], in1=tri_b, op=ALU.mult),
                         r=[rPT[m][buf], rC], w=[rPT[m][buf]])
            for m in range(2):
                for j in range(j0, 4):
                    oap, ob, first = oacc(m, j)
                    P.op("pe", lambda e, m=m, j=j, buf=buf, oap=oap, kb=kb, h=h, first=first, g=g: e.matmul(oap, lhsT=PT[m][buf][:, j * 128:(j + 1) * 128], rhs=VA[:, kb, h, :],
                                                                                                      start=(kb == 0 and first), stop=(kb == 4 * g + j), skip_group_check=True),
                         r=[rPT[m][buf], rV[kb]], w=[RB[ob]])

        def epilogue(g, h):
            for j in range(4):
                o1, b1_, _ = oacc(0, j)
                o2, b2_, _ = oacc(1, j)
                w_a, w_b = ew[ecount[0] % 2]
                re_ = rEw[ecount[0] % 2]
                ecount[0] += 1
                P.op("dve", lambda e, o1=o1: e.reciprocal(out=est[:, 0:1], in_=o1[:, 128:129]), r=[RB[b1_]], w=[rEst])
                P.op("dve", lambda e, o2=o2: e.reciprocal(out=est[:, 1:2], in_=o2[:, 128:129]), r=[RB[b2_]], w=[rEst])
                P.op("dve", lambda e: e.tensor_tensor(out=est[:, 1:2], in0=est[:, 1:2], in1=neglam[:, 0:1], op=ALU.mult), r=[rEst, rC], w=[rEst])
                P.op("act", lambda e, o1=o1, w_a=w_a: e.activation(out=w_a[:, 0:128], in_=o1[:, 0:128], func=AF.Identity, scale=est[:, 0:1]), r=[RB[b1_], rEst], w=[re_])
                P.op("dve", lambda e, o2=o2, w_a=w_a: e.scalar_tensor_tensor(out=w_a[:, 0:128], in0=o2[:, 0:128], scalar=est[:, 1:2], in1=w_a[:, 0:128], op0=ALU.mult, op1=ALU.add),
                     r=[RB[b2_], rEst, re_], w=[re_])
                P.op("act", lambda e, w_a=w_a, w_b=w_b: e.activation(out=w_b[:, 0:128], in_=w_a[:, 0:128], func=AF.Square, accum_out=est[:, 2:3]), r=[re_], w=[re_, rEst])
                P.op("dve", lambda e: e.tensor_scalar(out=est[:, 3:4], in0=est[:, 2:3], scalar1=1.0 / 128, scalar2=EPS, op0=ALU.mult, op1=ALU.add), r=[rEst], w=[rEst])
                P.op("act", lambda e: e.activation(out=est[:, 4:5], in_=est[:, 3:4], func=AF.Sqrt), r=[rEst], w=[rEst])
                P.op("dve", lambda e: e.reciprocal(out=est[:, 5:6], in_=est[:, 4:5]), r=[rEst], w=[rEst])
                P.op("dve", lambda e, w_a=w_a, j=j, h=h: e.scalar_tensor_tensor(out=atok[j][:, h * 128:(h + 1) * 128], in0=w_a[:, 0:128], scalar=est[:, 5:6], in1=subg_bc[:, 0:128],
                                                                             op0=ALU.mult, op1=ALU.mult), r=[re_, rEst, rC], w=[rAt[j]])

        def outproj(g):
            for j in range(4):
                T = 4 * g + j
                for c in range(4):
                    P.op("pe", lambda e, j=j, c=c: e.transpose(psb(7, 128, BF16, off=c * 128), atok[j][:, c * 128:(c + 1) * 128], ident_b), r=[rAt[j], rC], w=[RB[7]])
                P.op("act", lambda e: e.activation(out=catT.rearrange("p c n -> p (c n)"), in_=psb(7, 512, BF16), func=AF.Identity), r=[RB[7]], w=[rCat])
                xt = xr[T % 2]
                P.dma(lambda e, xt=xt, T=T: e.dma_start(out=xt, in_=x_d[T * 128:(T + 1) * 128, :]), w=[rXr[T % 2]])
                xo = x1o[T % 2]
                for sl in range(2):
                    for c in range(8):
                        if c < 4:
                            lhsT = catT[:, c, :]
                            rr = [rCat, rWo]
                        else:
                            lhsT = ssl[g % 2][:, c - 4, j * 128:(j + 1) * 128]
                            rr = [rSsl[g % 2], rWo]
                        P.op("pe", lambda e, lhsT=lhsT, c=c, sl=sl: e.matmul(psb(7), lhsT=lhsT, rhs=Wo[:, c, sl * 512:(sl + 1) * 512], start=(c == 0), stop=(c == 7)), r=rr, w=[RB[7]])
                    P.op("dve", lambda e, xo=xo, sl=sl: e.tensor_tensor(out=xo[:, sl * 512:(sl + 1) * 512], in0=psb(7), in1=g1_bc[:, sl * 512:(sl + 1) * 512], op=ALU.mult), r=[RB[7], rC], w=[rX1[T % 2]])
                P.op("pool", lambda e, xo=xo, xt=xt: e.tensor_tensor(out=xo, in0=xo, in1=xt, op=ALU.add), r=[rX1[T % 2], rXr[T % 2]], w=[rX1[T % 2]])
                P.dma(lambda e, xo=xo, T=T: e.dma_start(out=x1s_d[T * 128:(T + 1) * 128, :], in_=xo), r=[rX1[T % 2]], w=[rSSO])
                if dbg and T == 5:
                    da = dout("dbg_atok", [128, 512], BF16)
                    P.dma(lambda e, j=j: e.dma_start(out=da, in_=atok[j]), r=[rAt[j]])
        emit_S(0)
        for i, (g, h, kb) in enumerate(steps):
            if i + 1 < len(steps):
                emit_S(i + 1)
            emit_PV(i)
            if kb == 4 * g + 3:
                epilogue(g, h)
                if h == 3:
                    outproj(g)
        P.barrier()
        if stop == 7:
            return finish()
        if dbg:
            dx1 = dout("dbg_x1", [256, D])
            P.dma(lambda e: e.dma_start(out=dx1, in_=x1s_d[512:768, :]))
            P.barrier()

        h2T = A(24, 16384, BF16).rearrange("p (k n) -> p k n", k=8)
        h2f = A(40, 16384).rearrange("p (k n) -> p k n", k=8)
        acc = A(56, 32768).rearrange("p (t n) -> p t n", t=8)
        aT = A(88, 16384, BF16).rearrange("p (k n) -> p k n", k=8)
        W2b = [A(104 + 16 * i, 16384, BF16).rearrange("p (k n) -> p k n", k=8) for i in range(2)]
        w2st = [A(136 + 4 * i, 4096) for i in range(2)]
        w1st = [A(144 + 8 * i, 8192).rearrange("p (a k n) -> p a k n", a=2, k=8) for i in range(2)]
        W1t = [A(160 + 4 * i, 4096, BF16).rearrange("p (a k n) -> p a k n", a=2, k=8) for i in range(2)]
        wk = [[A(168 + 6 * i + 2 * k_, 2048) for k_ in range(3)] for i in range(2)]
        gates = A(180, 1024).rearrange("p (t n) -> p t n", t=8)
        xr = [A(181 + 4 * i, 4096) for i in range(2)]
        xsr = [A(189 + 4 * i, 4096) for i in range(2)]
        lgw = A(197, 1024)
        stat1 = A(198, 64)
        gTb = A(198.5, 256, BF16)
        gates_b = A(198.75, 512, BF16).rearrange("p (t n) -> p t n", t=8)
        b2b = A(199.25, 2048, BF16)
        P.op("dve", lambda e: e.tensor_copy(out=b2b[0:32, :], in_=b2t[0:32, :]), r=[rC], w=[rC])
        rXr, rXs, rStat, rH2, rH2f, rAcc, raT, rW2b, rW2s, rW1s, rW1t, rWk, rGates, rLg, rgT = \
            RL(2), RL(2), RL(4), RL(8), RL(8), RL(8), RL(8), RL(2), RL(2), RL(2), RL(2), RL(2, 3), R(), R(), R()
        w1cnt = 0
        wkcnt = 0

        def load_w1(idx, ee, fc):
            stg = w1st[idx % 2]
            w1t = W1t[idx % 2]
            rs_, rt_ = rW1s[idx % 2], rW1t[idx % 2]
            for a in range(2):
                c0 = a * 1024 + fc * 128
                P.dma(lambda e, stg=stg, ee=ee, a=a, c0=c0: e.dma_start(out=stg[:, a, :, :], in_=w1_d[ee, :, c0:c0 + 128].rearrange("(k p) n -> p k n", p=128)), w=[rs_])
            P.op("act", lambda e, stg=stg, w1t=w1t: e.activation(out=w1t.rearrange("p a k n -> p (a k n)"), in_=stg.rearrange("p a k n -> p (a k n)"), func=AF.Identity), r=[rs_], w=[rt_])

        def w2_dma(ee, k):
            P.dma(lambda e, ee=ee, k=k: e.dma_start(out=w2st[k % 2], in_=w2_d[ee, k * 128:(k + 1) * 128, :]), w=[rW2s[k % 2]])

        def w2_cast(ee, k):
            P.op("act", lambda e, ee=ee, k=k: e.activation(out=W2b[ee % 2][:, k, :], in_=w2st[k % 2], func=AF.Identity), r=[rW2s[k % 2]], w=[rW2b[ee % 2]])
        if dbg:
            dlg = dout("dbg_gates", [128, 8, 32])
        for qt in range(nqt):
            for half in range(2):
                def ev(kc, half=half):
                    P.op("act", lambda e, kc=kc: e.activation(out=h2f[:, kc, :], in_=psb(kc), func=AF.Identity, scale=A2c[:, kc:kc + 1], bias=B2c[:, kc:kc + 1]), r=[RB[kc], rC], w=[rH2f[kc]])
                    P.op("dve", lambda e, kc=kc, half=half: e.tensor_copy(out=h2T[:, kc, half * 512:(half + 1) * 512], in_=h2f[:, kc, :]), r=[rH2f[kc]], w=[rH2[kc]])
                norm_T(x1s_d, qt * 1024 + half * 512, xr, xsr, A2c, B2c, ev, rXr, rXs, stat1, rStat)
                for i in range(4):
                    t = half * 4 + i
                    for kc in range(8):
                        P.op("pe", lambda e, kc=kc, i=i: e.matmul(psb(0, 32), lhsT=h2f[:, kc, i * 128:(i + 1) * 128], rhs=wr_t.rearrange("p (k n) -> p k n", k=8)[:, kc, :], start=(kc == 0), stop=(kc == 7)),
                             r=[rH2f[kc], rC], w=[RB[0]])
                    lg, ex, mk, t8 = lgw[:, 0:32], lgw[:, 32:64], lgw[:, 64:96], lgw[:, 96:104]
                    P.op("dve", lambda e, lg=lg: e.tensor_tensor(out=lg, in0=psb(0, 32), in1=brt_bc[:, 0:32], op=ALU.add), r=[RB[0], rC], w=[rLg])
                    P.op("dve", lambda e, lg=lg, t8=t8: e.max(out=t8, in_=lg), r=[rLg], w=[rLg])
                    P.op("dve", lambda e, lg=lg, mk=mk, t8=t8: e.tensor_scalar(out=mk, in0=lg, scalar1=t8[:, 3:4], scalar2=None, op0=ALU.is_ge), r=[rLg], w=[rLg])
                    P.op("dve", lambda e, t8=t8: e.tensor_scalar(out=lgw[:, 128:129], in0=t8[:, 0:1], scalar1=-1.0, scalar2=None, op0=ALU.mult), r=[rLg], w=[rLg])
                    P.op("act", lambda e, lg=lg, ex=ex: e.activation(out=ex, in_=lg, func=AF.Exp, bias=lgw[:, 128:129]), r=[rLg], w=[rLg])
                    P.op("dve", lambda e, ex=ex, mk=mk: e.scalar_tensor_tensor(out=ex, in0=ex, scalar=1.0, in1=mk, op0=ALU.mult, op1=ALU.mult, accum_out=lgw[:, 129:130]), r=[rLg], w=[rLg])
                    P.op("dve", lambda e: e.reciprocal(out=lgw[:, 130:131], in_=lgw[:, 129:130]), r=[rLg], w=[rLg])
                    P.op("dve", lambda e, ex=ex, t=t: e.tensor_scalar(out=gates[:, t, :], in0=ex, scalar1=lgw[:, 130:131], scalar2=None, op0=ALU.mult), r=[rLg], w=[rGates])
            if dbg and qt == 0:
                P.dma(lambda e: e.dma_start(out=dlg, in_=gates), r=[rGates])
            for t in range(8):
                P.op("pool", lambda e, t=t: e.memset(acc[:, t, :], 0.0), w=[rAcc[t]])
            for ex_ in range(nexp):
                w2b = W2b[ex_ % 2]
                if ex_ == 0:
                    for k in range(8):
                        w2_dma(0, k)
                        w2_cast(0, k)
                for fc in range(8):
                    if ex_ == 0 and fc == 0:
                        load_w1(w1cnt, 0, 0)
                    w1t = W1t[w1cnt % 2]
                    rt_ = rW1t[w1cnt % 2]
                    w1cnt += 1
                    if fc < 7:
                        load_w1(w1cnt, ex_, fc + 1)
                    elif ex_ + 1 < nexp:
                        load_w1(w1cnt, ex_ + 1, 0)
                    if ex_ + 1 < nexp:
                        if fc > 0:
                            w2_cast(ex_ + 1, fc - 1)
                        w2_dma(ex_ + 1, fc)
                    for th in range(2):
                        bG, bU = (wkcnt % 2) * 2, (wkcnt % 2) * 2 + 1
                        wa, wb, wc = wk[wkcnt % 2]
                        rk = rWk[wkcnt % 2]
                        wkcnt += 1
                        for a, bb in ((0, bG), (1, bU)):
                            for kc in range(8):
                                P.op("pe", lambda e, a=a, bb=bb, kc=kc, w1t=w1t, th=th: e.matmul(psb(bb), lhsT=w1t[:, a, kc, :], rhs=h2T[:, kc, th * 512:(th + 1) * 512], start=(kc == 0), stop=(kc == 7)),
                                     r=[rt_, rH2[kc]], w=[RB[bb]])
                        cg = ex_ * 16 + fc
                        cu = ex_ * 16 + 8 + fc
                        rka, rkb, rkc = rk
                        P.op("dve", lambda e, wa=wa, bG=bG, cg=cg: e.tensor_scalar(out=wa[:, 0:512], in0=psb(bG), scalar1=b1cols[:, cg:cg + 1], scalar2=7.0, op0=ALU.add, op1=ALU.min), r=[RB[bG], rC], w=[rka])
                        P.op("act", lambda e, wa=wa, wb=wb: e.activation(out=wb[:, 0:512], in_=wa[:, 0:512], func=AF.Sigmoid, scale=1.702), r=[rka], w=[rkb])
                        P.op("pool", lambda e, wa=wa, wb=wb: e.tensor_tensor(out=wb[:, 0:512], in0=wb[:, 0:512], in1=wa[:, 0:512], op=ALU.mult), r=[rka, rkb], w=[rkb])
                        P.op("dve", lambda e, wc=wc, bU=bU, cu=cu: e.tensor_scalar(out=wc[:, 0:512], in0=psb(bU), scalar1=b1p1[:, cu:cu + 1], scalar2=8.0, op0=ALU.add, op1=ALU.min), r=[RB[bU], rC], w=[rkc])
                        P.op("dve", lambda e, wc=wc, wb=wb, fc=fc, th=th: e.scalar_tensor_tensor(out=aT[:, fc, th * 512:(th + 1) * 512], in0=wc[:, 0:512], scalar=-6.0, in1=wb[:, 0:512], op0=ALU.max, op1=ALU.mult),
                             r=[rkb, rkc], w=[raT[fc]])
                if ex_ + 1 < nexp:
                    w2_cast(ex_ + 1, 7)
                for t in range(8):
                    for sl in range(2):
                        bY = 4 + (t * 2 + sl) % 4
                        for fc in range(8):
                            P.op("pe", lambda e, t=t, sl=sl, fc=fc, bY=bY, w2b=w2b: e.matmul(psb(bY), lhsT=aT[:, fc, t * 128:(t + 1) * 128], rhs=w2b[:, fc, sl * 512:(sl + 1) * 512], start=(fc == 0), stop=(fc == 7)),
                                 r=[raT[fc], rW2b[ex_ % 2]], w=[RB[bY]])
                        P.op("dve", lambda e, t=t, sl=sl, bY=bY, ex_=ex_: e.scalar_tensor_tensor(out=acc[:, t, sl * 512:(sl + 1) * 512], in0=psb(bY), scalar=gates[:, t, ex_:ex_ + 1], in1=acc[:, t, sl * 512:(sl + 1) * 512],
                                                                                             op0=ALU.mult, op1=ALU.add), r=[RB[bY], rGates, rAcc[t]], w=[rAcc[t]])
            for t in range(8):
                T = qt * 8 + t
                if t == 0:
                    P.op("dve", lambda e: e.tensor_copy(out=gates_b, in_=gates), r=[rGates], w=[rGates])
                P.op("pe", lambda e, t=t: e.transpose(ps[0:32, 0, :].bitcast(BF16)[:, 0:128], gates_b[:, t, :], ident_b), r=[rGates, rC], w=[RB[0]])
                P.op("act", lambda e: e.activation(out=gTb[0:32, 0:128], in_=ps[0:32, 0, :].bitcast(BF16)[:, 0:128], func=AF.Identity), r=[RB[0]], w=[rgT])
                xt = xr[t % 2]
                xo = xsr[t % 2]
                P.dma(lambda e, xt=xt, T=T: e.dma_start(out=xt, in_=x1s_d[T * 128:(T + 1) * 128, :]), w=[rXr[t % 2]])
                for sl in range(2):
                    bb = 1 + sl
                    P.op("pe", lambda e, sl=sl, bb=bb: e.matmul(psb(bb), lhsT=gTb[0:32, 0:128], rhs=b2b[0:32, sl * 512:(sl + 1) * 512], start=True, stop=True), r=[rgT, rC], w=[RB[bb]])
                    P.op("dve", lambda e, t=t, sl=sl, bb=bb: e.tensor_tensor(out=acc[:, t, sl * 512:(sl + 1) * 512], in0=acc[:, t, sl * 512:(sl + 1) * 512], in1=psb(bb), op=ALU.add), r=[RB[bb], rAcc[t]], w=[rAcc[t]])
                P.op("pool", lambda e, t=t: e.tensor_tensor(out=acc[:, t, :], in0=acc[:, t, :], in1=g2_bc, op=ALU.mult), r=[rAcc[t], rC], w=[rAcc[t]])
                P.op("dve", lambda e, t=t, xt=xt: e.tensor_tensor(out=xt, in0=acc[:, t, :], in1=xt, op=ALU.add), r=[rAcc[t], rXr[t % 2]], w=[rXr[t % 2]])
                P.op("act", lambda e, xt=xt, xo=xo: e.activation(out=xo, in_=xt, func=AF.Square, accum_out=stat1[:, 0:1]), r=[rXr[t % 2]], w=[rXs[t % 2], rStat[0]])
                P.op("dve", lambda e: e.tensor_scalar(out=stat1[:, 1:2], in0=stat1[:, 0:1], scalar1=1.0 / D, scalar2=EPS, op0=ALU.mult, op1=ALU.add), r=[rStat[0]], w=[rStat[0]])
                P.op("act", lambda e: e.activation(out=stat1[:, 2:3], in_=stat1[:, 1:2], func=AF.Sqrt), r=[rStat[0]], w=[rStat[0]])
                P.op("dve", lambda e: e.reciprocal(out=stat1[:, 3:4], in_=stat1[:, 2:3]), r=[rStat[0]], w=[rStat[0]])
                P.op("dve", lambda e, xt=xt, xo=xo: e.scalar_tensor_tensor(out=xo, in0=xt, scalar=stat1[:, 3:4], in1=fg_bc, op0=ALU.mult, op1=ALU.mult), r=[rXr[t % 2], rStat, rC], w=[rXs[t % 2]])
                P.dma(lambda e, xo=xo, T=T: e.dma_start(out=out_d[T * 128:(T + 1) * 128, :], in_=xo), r=[rXs[t % 2]], w=[rSSO])
            P.barrier()
        with nc.Block() as block:
            P.replay(block)
    return nc


_CACHE = {}


def _prep_inputs(inp, b):
    f = lambda a: np.ascontiguousarray(np.asarray(a, dtype=np.float32))
    m = {
        "x": f(inp["x"][b]), "c": f(inp["c"][b]),
        "w_ada": f(inp["w_ada"][0]), "b_ada": f(inp["b_ada"][0]), "norm1_g": f(inp["norm1_g"][0]),
        "w_in": f(inp["w_in"][0]),
        "lqk": f(np.stack([inp["lq1"][0], inp["lk1"][0], inp["lq2"][0], inp["lk2"][0]], 0)),
        "subln_g": f(inp["subln_g"][0]),
        "ssm_a_re": f(inp["ssm_a_re"][0]), "ssm_a_im": f(inp["ssm_a_im"][0]), "ssm_log_dt": f(inp["ssm_log_dt"][0]),
        "ssm_b_re": f(inp["ssm_b_re"][0]), "ssm_b_im": f(inp["ssm_b_im"][0]),
        "ssm_c_re": f(inp["ssm_c_re"][0]), "ssm_c_im": f(inp["ssm_c_im"][0]),
        "ssm_d": f(inp["ssm_d"][0]), "w_glu": f(inp["w_glu"][0]), "b_glu": f(inp["b_glu"][0]),
        "w_out": f(inp["w_out"][0]), "norm2_g": f(inp["norm2_g"][0]),
        "w_router": f(inp["w_router"][0]), "b_router": f(inp["b_router"][0]),
        "w1": f(inp["w1"][0]), "b1": f(inp["b1"][0]), "w2": f(inp["w2"][0]), "b2": f(inp["b2"][0]),
        "final_g": f(inp["final_g"]),
    }
    return m


def kernel(**inputs):
    if "nc" not in _CACHE:
        _CACHE["nc"] = build(False)
    nc = _CACHE["nc"]
    in_maps = [_prep_inputs(inputs, b) for b in range(8)]
    res = run_bass_kernel_spmd(nc, in_maps, core_ids=list(range(8)))
    out = np.stack([np.asarray(r["out"], dtype=np.float32) for r in res.results], 0)
    return out
```

```python
import math
from contextlib import ExitStack

import numpy as np
import concourse.bass as bass
import concourse.mybir as mybir
from concourse.bass_utils import run_bass_kernel_spmd

F32 = mybir.dt.float32
BF16 = mybir.dt.bfloat16
I32 = mybir.dt.int32
AF = mybir.ActivationFunctionType
ALU = mybir.AluOpType

S = 4096
D = 1024
NT = 32
NE = 32
EPS = 1e-6
PI = math.pi
KS = [0, 1, 2, 3, 4, 5, 6, 7, 8, 16, 32, 64, 128, 256, 512, 1024, 2048]
NK = len(KS)
LAMBDA_INIT = 0.8 - 0.6 * math.exp(0.0)


class R:
    __slots__ = ("w", "rs")

    def __init__(self):
        self.w = None
        self.rs = []


def RL(*dims):
    if not dims:
        return R()
    return [RL(*dims[1:]) for _ in range(dims[0])]


def flat(x):
    if isinstance(x, R):
        return [x]
    out = []
    for e in x:
        out.extend(flat(e))
    return out


class Prog:
    ENG = ("pe", "act", "dve", "pool", "sp")
    CE = ("pe", "act", "dve", "pool")

    def __init__(self, nc, stack, n_lanes=32):
        self.nc = nc
        self.stack = stack
        self.seg = 0
        self.sems = {}
        self.sem = {e: self._newsem(e) for e in self.CE}
        self.lanes = [stack.enter_context(nc.semaphore(f"s_dma{i}")) for i in range(n_lanes)]
        self.lane_val = [0] * n_lanes
        self.lane_next = 0
        self.cnt = {e: 0 for e in self.CE}
        self.q = {e: [] for e in self.ENG}
        self.seen = {e: {} for e in self.ENG}

    def _need(self, e, dep, waits, force=False):
        if dep is None:
            return
        key, val = dep
        if isinstance(key[0], str) and key[0] != "L":
            if key[1] < self.seg:
                return
            if key[0] == "pe" and e == "pe" and not force:
                return
        if self.seen[e].get(key, 0) >= val:
            return
        self.seen[e][key] = val
        waits.append((key, val))

    def _deps(self, e, r, w):
        waits = []
        for x in flat(r):
            self._need(e, x.w, waits)
        for x in flat(w):
            self._need(e, x.w, waits)
            for d in x.rs:
                self._need(e, d, waits)
        return waits

    def _commit(self, me, r, w):
        for x in flat(r):
            x.rs.append(me)
        for x in flat(w):
            x.w = me
            x.rs = []

    def op(self, e, fn, r=(), w=()):
        waits = self._deps(e, r, w)
        self.cnt[e] += 1
        me = ((e, self.seg), self.cnt[e])
        self.q[e].append((waits, fn, ((e, self.seg), 1)))
        self._commit(me, r, w)

    def dma(self, fn, r=(), w=(), e="sp"):
        waits = self._deps(e, r, w)
        li = self.lane_next
        self.lane_next = (li + 1) % len(self.lanes)
        if self.lane_val[li] > 0:
            self._need(e, (("L", li), self.lane_val[li]), waits)
        self.lane_val[li] += 16
        me = (("L", li), self.lane_val[li])
        self.q[e].append((waits, fn, (("L", li), 16)))
        self._commit(me, r, w)

    def barrier(self):
        for e in self.ENG:
            waits = []
            for f in self.CE:
                if self.cnt[f] > 0:
                    self._need(e, ((f, self.seg), self.cnt[f]), waits, force=True)
            for li, v in enumerate(self.lane_val):
                if v > 0:
                    self._need(e, (("L", li), v), waits)
            if waits:
                self.q[e].append((waits, None, None))
        self.seg += 1
        for f in self.CE:
            self.cnt[f] = 0
            self._newsem(f)

    def _newsem(self, e):
        k = (e, self.seg)
        self.sems[k] = self.stack.enter_context(self.nc.semaphore(f"s_{e}_{self.seg}"))
        return self.sems[k]

    def _semof(self, key):
        if key[0] == "L":
            return self.lanes[key[1]]
        return self.sems[key]

    def replay(self, block):
        def mk(e):
            def body(engine):
                for waits, fn, inc in self.q[e]:
                    for key, val in waits:
                        engine.wait_ge(self._semof(key), val)
                    if fn is not None:
                        ins = fn(engine)
                        ins.then_inc(self._semof(inc[0]), inc[1])
            return body
        block.tensor(mk("pe"))
        block.scalar(mk("act"))
        block.vector(mk("dve"))
        block.gpsimd(mk("pool"))
        block.sync(mk("sp"))


def build(dbg=False, stop=None, nexp=NE, nqt=4, cut=99):
    nc = bass.Bass("TRN2", target_bir_lowering=False)
    dram = {}

    def din(name, shape):
        dram[name] = nc.dram_tensor(name, list(shape), F32, kind="ExternalInput").ap()
        return dram[name]

    x_d = din("x", [S, D])
    c_d = din("c", [D])
    wada_d = din("w_ada", [D, 6 * D])
    bada_d = din("b_ada", [6 * D])
    n1g_d = din("norm1_g", [D])
    win_d = din("w_in", [D, 2048])
    lqk_d = din("lqk", [4, 64])
    subg_d = din("subln_g", [128])
    are_d = din("ssm_a_re", [32, 64])
    aim_d = din("ssm_a_im", [32, 64])
    ldt_d = din("ssm_log_dt", [32])
    bre_d = din("ssm_b_re", [32, 64, 16])
    bim_d = din("ssm_b_im", [32, 64, 16])
    cre_d = din("ssm_c_re", [32, 16, 64])
    cim_d = din("ssm_c_im", [32, 16, 64])
    sd_d = din("ssm_d", [512])
    wglu_d = din("w_glu", [512, 512])
    bglu_d = din("b_glu", [512])
    wout_d = din("w_out", [D, D])
    n2g_d = din("norm2_g", [D])
    wr_d = din("w_router", [D, NE])
    br_d = din("b_router", [NE])
    full = stop is None or stop >= 9
    w1_d = din("w1", [nexp, D, 2048]) if full else None
    b1_d = din("b1", [NE, 2048])
    w2_d = din("w2", [nexp, D, D]) if full else None
    b2_d = din("b2", [NE, D])
    fg_d = din("final_g", [D])
    out_d = nc.dram_tensor("out", [S, D], F32, kind="ExternalOutput").ap()
    x1s_d = nc.dram_tensor("x1s", [S, D], F32, kind="Internal").ap()
    sso_d = nc.dram_tensor("ssos", [4, 128, S], BF16, kind="Internal").ap()
    dbg_d = {}

    def dout(name, shape, dt=F32):
        dbg_d[name] = nc.dram_tensor(name, list(shape), dt, kind="ExternalOutput").ap()
        return dbg_d[name]

    with ExitStack() as st:
        st.enter_context(nc.allow_non_contiguous_dma(reason="param layouts"))
        st.enter_context(nc.allow_low_precision(reason="bf16 matmul operands, fp32 accumulation"))
        AR = 204
        arena = st.enter_context(nc.sbuf_tensor("arena", [128, AR * 256], F32))
        ps = st.enter_context(nc.psum_tensor("ps", [128, 8, 512], F32))
        P = Prog(nc, st)
        RB = RL(8)

        def finish():
            P.barrier()
            P.dma(lambda e: e.dma_start(out=out_d[0:128, 0:128], in_=arena[:, 0:128]))
            P.barrier()
            with nc.Block() as block:
                P.replay(block)
            return nc

        def A(off_kib, nbytes, dt=F32, parts=128):
            o = int(round(off_kib * 256))
            n32 = (nbytes + 3) // 4
            assert o + n32 <= AR * 256, (off_kib, nbytes)
            v = arena[0:parts, o:o + n32]
            if dt != F32:
                v = v.bitcast(dt)
            return v

        def psb(b, n=512, dt=F32, off=0):
            if dt == F32:
                return ps[:, b, off:off + n]
            return ps[:, b, :].bitcast(dt)[:, off:off + n]

        cb = [0.0]

        def C(nbytes, dt=F32, parts=128):
            v = A(cb[0], nbytes, dt, parts)
            cb[0] += ((nbytes + 63) // 64) * 64 / 1024.0
            return v

        ident_f = C(512)
        J_f = C(512)
        ident_b = C(256, BF16)
        tri_b = C(256, BF16)
        g1_bc = C(4096)
        g2_bc = C(4096)
        fg_bc = C(4096)
        subg_bc = C(512)
        b2t = C(4096)
        b1cols = C(2048)
        b1p1 = C(2048)
        wr_t = C(1024)
        A1c, B1c, A2c, B2c = C(32), C(32), C(32), C(32)
        brt_bc = C(128)
        neglam = C(4)
        bglu_c = C(16)
        sgn = C(4)
        nsgn = C(4)
        epsc = C(4)
        assert cb[0] <= 24.0, cb[0]
        rC = R()

        P.op("pool", lambda e: e.memset(ident_f, 0.0), w=[rC])
        P.op("pool", lambda e: e.affine_select(out=ident_f, in_=ident_f, pattern=[[-1, 128]], compare_op=ALU.not_equal,
                                               fill=1.0, base=0, channel_multiplier=1), r=[rC], w=[rC])
        P.op("pool", lambda e: e.memset(J_f, 0.0), w=[rC])
        P.op("pool", lambda e: e.affine_select(out=J_f[:, 64:128], in_=J_f[:, 64:128], pattern=[[-1, 64]], compare_op=ALU.not_equal,
                                               fill=1.0, base=0, channel_multiplier=1), r=[rC], w=[rC])
        P.op("pool", lambda e: e.affine_select(out=J_f[:, 0:64], in_=J_f[:, 0:64], pattern=[[-1, 64]], compare_op=ALU.not_equal,
                                               fill=1.0, base=-64, channel_multiplier=1), r=[rC], w=[rC])
        P.op("pool", lambda e: e.tensor_copy(out=ident_b, in_=ident_f), r=[rC], w=[rC])
        P.op("pool", lambda e: e.memset(tri_b, 1.0), w=[rC])
        P.op("pool", lambda e: e.affine_select(out=tri_b, in_=tri_b, pattern=[[1, 128]], compare_op=ALU.is_ge,
                                               fill=0.0, base=0, channel_multiplier=-1), r=[rC], w=[rC])
        P.op("pool", lambda e: e.memset(sgn[0:64, :], -1.0), w=[rC])
        P.op("pool", lambda e: e.memset(sgn[64:128, :], 1.0), w=[rC])
        P.op("pool", lambda e: e.memset(nsgn[0:64, :], 1.0), w=[rC])
        P.op("pool", lambda e: e.memset(nsgn[64:128, :], -1.0), w=[rC])
        P.op("pool", lambda e: e.memset(epsc, EPS), w=[rC])
        P.dma(lambda e: e.dma_start(out=fg_bc, in_=fg_d.partition_broadcast(128)), w=[rC])
        P.dma(lambda e: e.dma_start(out=subg_bc[:, 0:128], in_=subg_d.partition_broadcast(128)), w=[rC])
        P.dma(lambda e: e.dma_start(out=b2t[0:32, :], in_=b2_d), w=[rC])
        P.dma(lambda e: e.dma_start(out=wr_t.rearrange("p (k n) -> p k n", k=8), in_=wr_d.rearrange("(k p) n -> p k n", p=128)), w=[rC])
        P.dma(lambda e: e.dma_start(out=brt_bc[:, 0:32], in_=br_d.partition_broadcast(128)), w=[rC])
        P.dma(lambda e: e.dma_start(out=bglu_c[:, 0:4], in_=bglu_d.rearrange("(k p) -> p k", p=128)), w=[rC])
        P.op("dve", lambda e: e.tensor_scalar(out=subg_bc[:, 0:128], in0=subg_bc[:, 0:128], scalar1=1.0 - LAMBDA_INIT, scalar2=None, op0=ALU.mult), r=[rC], w=[rC])

        o0 = 24.0
        mod_bc = A(o0, 24576)
        b_bc = A(o0 + 24, 24576)
        wts = [A(o0 + 48 + 16 * i, 16384).rearrange("p (k n) -> p k n", k=8) for i in range(2)]
        c_col = A(o0 + 80, 32)
        c_rep = A(o0 + 81, 4096).rearrange("p (k n) -> p k n", k=8)
        n1c = A(o0 + 85, 32)
        n2c = A(o0 + 85.5, 32)
        lq = A(o0 + 86, 1024).rearrange("p (a n) -> p a n", a=4)
        lqs = A(o0 + 87, 16)
        junk0 = A(o0 + 88, 512)
        b1s = A(o0 + 89, 2048)
        rwt = RL(2)
        rP0 = R()
        P.dma(lambda e: e.dma_start(out=c_col[:, 0:8], in_=c_d.rearrange("(k p) -> p k", p=128)), w=[rP0])
        P.dma(lambda e: e.dma_start(out=b_bc, in_=bada_d.partition_broadcast(128)), w=[rP0])
        P.dma(lambda e: e.dma_start(out=n1c[:, 0:8], in_=n1g_d.rearrange("(k p) -> p k", p=128)), w=[rP0])
        P.dma(lambda e: e.dma_start(out=n2c[:, 0:8], in_=n2g_d.rearrange("(k p) -> p k", p=128)), w=[rP0])
        P.dma(lambda e: e.dma_start(out=lq, in_=lqk_d.partition_broadcast(128)), w=[rP0])
        P.op("act", lambda e: e.activation(out=c_col[:, 0:8], in_=c_col[:, 0:8], func=AF.Silu), r=[rP0], w=[rP0])
        for k in range(8):
            P.op("dve", lambda e, k=k: e.tensor_copy(out=c_rep[:, k, :], in_=c_col[:, k:k + 1].to_broadcast([128, 128])), r=[rP0], w=[rP0])
        for ns in range(12):
            wt = wts[ns % 2]
            P.dma(lambda e, wt=wt, ns=ns: e.dma_start(out=wt, in_=wada_d[:, ns * 512:(ns + 1) * 512].rearrange("(k p) n -> p k n", p=128)), w=[rwt[ns % 2]])
            b = ns % 2
            for k in range(8):
                P.op("pe", lambda e, wt=wt, k=k, b=b: e.matmul(psb(b), lhsT=c_rep[:, k, :], rhs=wt[:, k, :], start=(k == 0), stop=(k == 7)),
                     r=[rP0, rwt[ns % 2]], w=[RB[b]])
            P.op("dve", lambda e, ns=ns, b=b: e.tensor_tensor(out=mod_bc[:, ns * 512:(ns + 1) * 512], in0=psb(b), in1=b_bc[:, ns * 512:(ns + 1) * 512], op=ALU.add),
                 r=[RB[b], rP0], w=[rP0])
        P.op("dve", lambda e: e.tensor_copy(out=g1_bc, in_=mod_bc[:, 2048:3072]), r=[rP0], w=[rC])
        P.op("dve", lambda e: e.tensor_copy(out=g2_bc, in_=mod_bc[:, 5120:6144]), r=[rP0], w=[rC])

        def diag_col(dst, base):
            for k in range(8):
                P.op("dve", lambda e, k=k: e.scalar_tensor_tensor(out=junk0, in0=mod_bc[:, base + k * 128:base + (k + 1) * 128], scalar=1.0, in1=ident_f,
                                                                   op0=ALU.mult, op1=ALU.mult, accum_out=dst[:, k:k + 1]), r=[rP0, rC], w=[rC])
        diag_col(B1c, 0)
        diag_col(A1c, 1024)
        diag_col(B2c, 3072)
        diag_col(A2c, 4096)
        P.op("dve", lambda e: e.scalar_tensor_tensor(out=A1c[:, 0:8], in0=A1c[:, 0:8], scalar=1.0, in1=n1c[:, 0:8], op0=ALU.add, op1=ALU.mult), r=[rP0, rC], w=[rC])
        P.op("dve", lambda e: e.scalar_tensor_tensor(out=A2c[:, 0:8], in0=A2c[:, 0:8], scalar=1.0, in1=n2c[:, 0:8], op0=ALU.add, op1=ALU.mult), r=[rP0, rC], w=[rC])
        P.op("dve", lambda e: e.scalar_tensor_tensor(out=junk0[:, 0:64], in0=lq[:, 0, :], scalar=1.0, in1=lq[:, 1, :], op0=ALU.mult, op1=ALU.mult, accum_out=lqs[:, 0:1]), r=[rP0], w=[rP0])
        P.op("dve", lambda e: e.scalar_tensor_tensor(out=junk0[:, 0:64], in0=lq[:, 2, :], scalar=1.0, in1=lq[:, 3, :], op0=ALU.mult, op1=ALU.mult, accum_out=lqs[:, 1:2]), r=[rP0], w=[rP0])
        P.op("act", lambda e: e.activation(out=lqs[:, 0:2], in_=lqs[:, 0:2], func=AF.Exp), r=[rP0], w=[rP0])
        P.op("dve", lambda e: e.tensor_tensor(out=lqs[:, 2:3], in0=lqs[:, 1:2], in1=lqs[:, 0:1], op=ALU.subtract), r=[rP0], w=[rP0])
        P.op("dve", lambda e: e.tensor_scalar(out=neglam[:, 0:1], in0=lqs[:, 2:3], scalar1=-LAMBDA_INIT, scalar2=None, op0=ALU.add), r=[rP0], w=[rC])
        b1v = b1_d.rearrange("e (k p) -> (e k) p", p=128)
        for i in range(4):
            P.dma(lambda e, i=i: e.dma_start(out=b1s[:, i * 128:(i + 1) * 128], in_=b1v[i * 128:(i + 1) * 128, :]), w=[rP0])
        for i in range(4):
            P.op("pe", lambda e, i=i: e.transpose(psb(2, 128, off=i * 128), b1s[:, i * 128:(i + 1) * 128], ident_f), r=[rP0, rC], w=[RB[2]])
        P.op("dve", lambda e: e.tensor_copy(out=b1cols[:, 0:512], in_=psb(2)), r=[RB[2]], w=[rC])
        P.op("dve", lambda e: e.tensor_scalar(out=b1p1[:, 0:512], in0=b1cols[:, 0:512], scalar1=1.0, scalar2=None, op0=ALU.add), r=[rC], w=[rC])
        P.barrier()
        if stop == 0:
            return finish()

        def norm_T(src_d, t0, xr, xsr, Acol, Bcol, evac_fn, rXr, rXs, stat, rStat):
            for pr in range(2):
                tiles = []
                for i in (2 * pr, 2 * pr + 1):
                    j = t0 // 128 + i
                    tiles.append((i, j, xr[j % 2], xsr[j % 2], 4 * (j % 4), rStat[j % 4]))
                for i, j, xt, xs, so, rS in tiles:
                    P.dma(lambda e, xt=xt, j=j: e.dma_start(out=xt, in_=src_d[j * 128:(j + 1) * 128, :]), w=[rXr[j % 2]])
                for i, j, xt, xs, so, rS in tiles:
                    P.op("act", lambda e, xt=xt, xs=xs, so=so: e.activation(out=xs, in_=xt, func=AF.Square, accum_out=stat[:, so:so + 1]), r=[rXr[j % 2]], w=[rXs[j % 2], rS])
                for i, j, xt, xs, so, rS in tiles:
                    P.op("dve", lambda e, so=so: e.tensor_scalar(out=stat[:, so + 1:so + 2], in0=stat[:, so:so + 1], scalar1=1.0 / D, scalar2=EPS, op0=ALU.mult, op1=ALU.add), r=[rS], w=[rS])
                for i, j, xt, xs, so, rS in tiles:
                    P.op("act", lambda e, so=so: e.activation(out=stat[:, so + 2:so + 3], in_=stat[:, so + 1:so + 2], func=AF.Sqrt), r=[rS], w=[rS])
                for i, j, xt, xs, so, rS in tiles:
                    P.op("dve", lambda e, so=so: e.reciprocal(out=stat[:, so + 3:so + 4], in_=stat[:, so + 2:so + 3]), r=[rS], w=[rS])
                for i, j, xt, xs, so, rS in tiles:
                    P.op("act", lambda e, xt=xt, xs=xs, so=so: e.activation(out=xs, in_=xt, func=AF.Identity, scale=stat[:, so + 3:so + 4]), r=[rXr[j % 2], rS], w=[rXs[j % 2]])
                for i, j, xt, xs, so, rS in tiles:
                    for kc in range(8):
                        P.op("pe", lambda e, xs=xs, kc=kc, i=i: e.transpose(psb(kc, 128, off=i * 128), xs[:, kc * 128:(kc + 1) * 128], ident_f),
                             r=[rXs[j % 2], rC], w=[RB[kc]])
            for kc in range(8):
                evac_fn(kc)

        Un = [A(24 + 8 * nt, 8192, BF16).rearrange("p (g s c) -> p g s c", g=32, s=8) for nt in range(4)]
        rUn = RL(4)
        wu_st = A(56, 16384).rearrange("p (k n) -> p k n", k=8)
        Wu = A(72, 8192, BF16).rearrange("p (k n) -> p k n", k=8)
        xr = [A(80 + 4 * i, 4096) for i in range(2)]
        xsr = [A(88 + 4 * i, 4096) for i in range(2)]
        h1g = A(96, 16384, BF16).rearrange("p (k n) -> p k n", k=8)
        stat1 = A(112, 64)
        rXr, rXs, rStat, rWu, rH = RL(2), RL(2), RL(4), R(), RL(8)
        P.dma(lambda e: e.dma_start(out=wu_st, in_=win_d[:, 1536:2048].rearrange("(k p) n -> p k n", p=128)), w=[rWu])
        P.op("pool", lambda e: e.tensor_copy(out=Wu, in_=wu_st), r=[rWu], w=[rWu])
        for tg in range(4):
            for half in range(2):
                def ev(kc, half=half):
                    eng = "act" if kc % 2 == 0 else "dve"
                    dst = h1g[:, kc, half * 512:(half + 1) * 512]
                    if eng == "act":
                        P.op("act", lambda e, kc=kc, dst=dst: e.activation(out=dst, in_=psb(kc), func=AF.Identity, scale=A1c[:, kc:kc + 1], bias=B1c[:, kc:kc + 1]),
                             r=[RB[kc], rC], w=[rH[kc]])
                    else:
                        P.op("dve", lambda e, kc=kc, dst=dst: e.tensor_scalar(out=dst, in0=psb(kc), scalar1=A1c[:, kc:kc + 1], scalar2=B1c[:, kc:kc + 1], op0=ALU.mult, op1=ALU.add),
                             r=[RB[kc], rC], w=[rH[kc]])
                norm_T(x_d, tg * 1024 + half * 512, xr, xsr, A1c, B1c, ev, rXr, rXs, stat1, rStat)
            for s in range(8):
                b = s % 2
                for kc in range(8):
                    lhsT = h1g[:, kc, :].rearrange("p (n s) -> p s n", s=8)[:, s, :]
                    P.op("pe", lambda e, lhsT=lhsT, kc=kc, b=b: e.matmul(psb(b), lhsT=lhsT, rhs=Wu[:, kc, :], start=(kc == 0), stop=(kc == 7)),
                         r=[rH[kc], rWu], w=[RB[b]])
                if s % 2 == 0:
                    P.op("act", lambda e, tg=tg, s=s, b=b: e.activation(out=Un[tg][:, :, s, :], in_=psb(b).rearrange("p (g c) -> p g c", g=32), func=AF.Identity), r=[RB[b]], w=[rUn[tg]])
                else:
                    P.op("dve", lambda e, tg=tg, s=s, b=b: e.tensor_copy(out=Un[tg][:, :, s, :], in_=psb(b).rearrange("p (g c) -> p g c", g=32)), r=[RB[b]], w=[rUn[tg]])
        P.barrier()
        if stop == 1:
            return finish()

        Ztok = [A(56 + 8 * nt, 8192, BF16).rearrange("p (t c) -> p t c", t=8) for nt in range(4)]
        Bpow = A(88, 8192, BF16).rearrange("p (g m) -> p g m", g=32)
        Cpw = A(96, 8192, BF16).rearrange("p (g t c) -> p g t c", g=32, t=8)
        Toep = A(104, 8192, BF16).rearrange("p (g m) -> p g m", g=32)
        TB = NK * 32 * 4
        pw_re = A(112, TB).rearrange("p (k g) -> p k g", k=NK)
        pw_im = A(114.25, TB).rearrange("p (k g) -> p k g", k=NK)
        PW2 = A(116.5, TB).rearrange("p (k g) -> p k g", k=NK)
        g0 = 120.0
        T1 = A(g0, TB).rearrange("p (k g) -> p k g", k=NK)
        T2 = A(g0 + 2.25, TB).rearrange("p (k g) -> p k g", k=NK)
        T3 = A(g0 + 4.5, TB).rearrange("p (k g) -> p k g", k=NK)
        T3i = A(g0 + 4.5, TB, I32).rearrange("p (k g) -> p k g", k=NK)
        PBs = A(g0 + 6.75, TB).rearrange("p (k g) -> p k g", k=NK)
        ardt = A(g0 + 9, 128)
        aidt = A(g0 + 9.25, 128)
        dtb = A(g0 + 9.5, 128)
        arr = A(g0 + 9.75, 128)
        aii = A(g0 + 10, 128)
        sm = [A(g0 + 10.25 + 0.125 * i, 128) for i in range(8)]
        bT = [A(g0 + 12 + 2 * i, 2048).rearrange("p (g c) -> p g c", g=32) for i in range(2)]
        cT = [A(g0 + 16 + 2 * i, 2048).rearrange("p (g c) -> p g c", g=32) for i in range(2)]
        Bb = [A(g0 + 20 + 2 * i, 2048).rearrange("p (g c) -> p g c", g=32) for i in range(2)]
        X1 = A(g0 + 24, 2048).rearrange("p (g c) -> p g c", g=32)
        X2 = A(g0 + 26, 2048).rearrange("p (g c) -> p g c", g=32)
        Y1 = A(g0 + 28, 2048).rearrange("p (g c) -> p g c", g=32)
        Y2 = A(g0 + 30, 2048).rearrange("p (g c) -> p g c", g=32)
        tmpA = A(g0 + 32, 2048).rearrange("p (g c) -> p g c", g=32)
        cin = A(g0 + 34, 512)
        BpT = A(g0 + 35, 16384).rearrange("p (g s c) -> p g s c", g=32, s=8)
        CpK = A(g0 + 51, 16384).rearrange("p (g t c) -> p g t c", g=32, t=8)
        Zt = [A(g0 + 67 + i, 960) for i in range(2)]
        dcol = A(g0 + 69, 128)
        rG = R()
        rW = R()

        for h in range(2):
            sl = slice(64 * h, 64 * h + 64)
            P.dma(lambda e, sl=sl: e.dma_start(out=arr[sl, 0:32], in_=are_d.rearrange("g p -> p g")), w=[rG])
            P.dma(lambda e, sl=sl: e.dma_start(out=aii[sl, 0:32], in_=aim_d.rearrange("g p -> p g")), w=[rG])
            P.dma(lambda e, sl=sl: e.dma_start(out=bT[0][sl], in_=bre_d.rearrange("g p c -> p g c")), w=[rG])
            P.dma(lambda e, sl=sl: e.dma_start(out=bT[1][sl], in_=bim_d.rearrange("g p c -> p g c")), w=[rG])
        P.dma(lambda e: e.dma_start(out=dtb[:, 0:32], in_=ldt_d.partition_broadcast(128)), w=[rG])
        for s in range(8):
            P.dma(lambda e, s=s: e.dma_start(out=dcol[16 * s:16 * s + 16, 0:32], in_=sd_d.rearrange("(g c) -> c g", c=16)), w=[rG])
        for ci, cd in enumerate((cre_d, cim_d)):
            cv = cd.rearrange("g c p -> (g c) p")
            for i in range(4):
                for h in range(2):
                    P.dma(lambda e, i=i, h=h, cv=cv: e.dma_start(out=cin[:, 64 * h:64 * h + 64], in_=cv[i * 128:(i + 1) * 128, :]), w=[rG])
                P.op("pe", lambda e, i=i: e.transpose(psb(3, 128, off=i * 128), cin[:, 0:128], ident_f), r=[rG, rC], w=[RB[3]])
            P.op("dve", lambda e, ci=ci: e.tensor_copy(out=cT[ci].rearrange("p g c -> p (g c)"), in_=psb(3)), r=[RB[3]], w=[rG])

        P.op("act", lambda e: e.activation(out=dtb[:, 0:32], in_=dtb[:, 0:32], func=AF.Exp), r=[rG], w=[rG])
        P.op("dve", lambda e: e.tensor_scalar(out=arr[:, 0:32], in0=arr[:, 0:32], scalar1=-1e-4, scalar2=None, op0=ALU.min), r=[rG], w=[rG])
        P.op("dve", lambda e: e.tensor_tensor(out=ardt[:, 0:32], in0=arr[:, 0:32], in1=dtb[:, 0:32], op=ALU.mult), r=[rG], w=[rG])
        P.op("dve", lambda e: e.tensor_tensor(out=aidt[:, 0:32], in0=aii[:, 0:32], in1=dtb[:, 0:32], op=ALU.mult), r=[rG], w=[rG])
        for ki, k in enumerate(KS):
            P.op("act", lambda e, ki=ki, k=k: e.activation(out=T1[:, ki, :], in_=ardt[:, 0:32], func=AF.Exp, scale=float(k)), r=[rG], w=[rG])
            P.op("dve", lambda e, ki=ki, k=k: e.tensor_scalar(out=T2[:, ki, :], in0=aidt[:, 0:32], scalar1=float(k), scalar2=None, op0=ALU.mult), r=[rG], w=[rG])
        fl = lambda t: t.rearrange("p k g -> p (k g)")

        def reduce_sin(dst, src):
            P.op("dve", lambda e: e.tensor_scalar(out=fl(T3), in0=fl(src), scalar1=1.0 / (2 * PI), scalar2=None, op0=ALU.mult), r=[rG], w=[rG])
            P.op("dve", lambda e: e.tensor_copy(out=fl(T3i), in_=fl(T3)), r=[rG], w=[rG])
            P.op("dve", lambda e: e.tensor_copy(out=fl(T3), in_=fl(T3i)), r=[rG], w=[rG])
            P.op("dve", lambda e: e.scalar_tensor_tensor(out=fl(T3), in0=fl(T3), scalar=-2 * PI, in1=fl(src), op0=ALU.mult, op1=ALU.add), r=[rG], w=[rG])
            P.op("dve", lambda e: e.tensor_scalar(out=fl(T3), in0=fl(T3), scalar1=PI, scalar2=-PI, op0=ALU.min, op1=ALU.max), r=[rG], w=[rG])
            P.op("act", lambda e: e.activation(out=fl(dst), in_=fl(T3), func=AF.Sin), r=[rG], w=[rG])
        reduce_sin(pw_im, T2)
        P.op("dve", lambda e: e.tensor_scalar(out=fl(T2), in0=fl(T2), scalar1=PI / 2, scalar2=None, op0=ALU.add), r=[rG], w=[rG])
        reduce_sin(pw_re, T2)
        P.op("dve", lambda e: e.tensor_tensor(out=fl(pw_re), in0=fl(pw_re), in1=fl(T1), op=ALU.mult), r=[rG], w=[rG])
        P.op("dve", lambda e: e.tensor_tensor(out=fl(pw_im), in0=fl(pw_im), in1=fl(T1), op=ALU.mult), r=[rG], w=[rG])
        P.op("dve", lambda e: e.tensor_scalar(out=fl(PBs), in0=fl(pw_im), scalar1=sgn[:, 0:1], scalar2=None, op0=ALU.mult), r=[rG, rC], w=[rG])
        P.op("dve", lambda e: e.tensor_scalar(out=fl(PW2), in0=fl(pw_im), scalar1=nsgn[:, 0:1], scalar2=None, op0=ALU.mult), r=[rG, rC], w=[rG])
        nr, ni, den, t0_, t1_, cre_, cim_, t2_ = [s_[:, 0:32] for s_ in sm]
        P.op("dve", lambda e: e.tensor_scalar(out=nr, in0=pw_re[:, 1, :], scalar1=-1.0, scalar2=None, op0=ALU.add), r=[rG], w=[rG])
        P.op("dve", lambda e: e.tensor_copy(out=ni, in_=pw_im[:, 1, :]), r=[rG], w=[rG])
        P.op("dve", lambda e: e.tensor_tensor(out=den, in0=arr[:, 0:32], in1=arr[:, 0:32], op=ALU.mult), r=[rG], w=[rG])
        P.op("dve", lambda e: e.tensor_tensor(out=t0_, in0=aii[:, 0:32], in1=aii[:, 0:32], op=ALU.mult), r=[rG], w=[rG])
        P.op("dve", lambda e: e.tensor_tensor(out=den, in0=den, in1=t0_, op=ALU.add), r=[rG], w=[rG])
        P.op("dve", lambda e: e.reciprocal(out=den, in_=den), r=[rG], w=[rG])
        P.op("dve", lambda e: e.tensor_tensor(out=t0_, in0=nr, in1=arr[:, 0:32], op=ALU.mult), r=[rG], w=[rG])
        P.op("dve", lambda e: e.tensor_tensor(out=t1_, in0=ni, in1=aii[:, 0:32], op=ALU.mult), r=[rG], w=[rG])
        P.op("dve", lambda e: e.tensor_tensor(out=t0_, in0=t0_, in1=t1_, op=ALU.add), r=[rG], w=[rG])
        P.op("dve", lambda e: e.tensor_tensor(out=cre_, in0=t0_, in1=den, op=ALU.mult), r=[rG], w=[rG])
        P.op("dve", lambda e: e.tensor_tensor(out=t0_, in0=ni, in1=arr[:, 0:32], op=ALU.mult), r=[rG], w=[rG])
        P.op("dve", lambda e: e.tensor_tensor(out=t1_, in0=nr, in1=aii[:, 0:32], op=ALU.mult), r=[rG], w=[rG])
        P.op("dve", lambda e: e.tensor_tensor(out=t0_, in0=t0_, in1=t1_, op=ALU.subtract), r=[rG], w=[rG])
        P.op("dve", lambda e: e.tensor_tensor(out=cim_, in0=t0_, in1=den, op=ALU.mult), r=[rG], w=[rG])
        bc16 = lambda v: v.rearrange("p (g o) -> p g o", o=1).to_broadcast([128, 32, 16])
        P.op("dve", lambda e: e.tensor_tensor(out=Bb[0], in0=bT[0], in1=bc16(cre_), op=ALU.mult), r=[rG], w=[rG])
        P.op("dve", lambda e: e.tensor_tensor(out=tmpA, in0=bT[1], in1=bc16(cim_), op=ALU.mult), r=[rG], w=[rG])
        P.op("dve", lambda e: e.tensor_tensor(out=Bb[0], in0=Bb[0], in1=tmpA, op=ALU.subtract), r=[rG], w=[rG])
        P.op("dve", lambda e: e.tensor_tensor(out=Bb[1], in0=bT[1], in1=bc16(cre_), op=ALU.mult), r=[rG], w=[rG])
        P.op("dve", lambda e: e.tensor_tensor(out=tmpA, in0=bT[0], in1=bc16(cim_), op=ALU.mult), r=[rG], w=[rG])
        P.op("dve", lambda e: e.tensor_tensor(out=Bb[1], in0=Bb[1], in1=tmpA, op=ALU.add), r=[rG], w=[rG])
        lo, hi = slice(0, 64), slice(64, 128)
        P.op("dve", lambda e: e.tensor_copy(out=X1[lo], in_=Bb[0][lo]), r=[rG], w=[rG])
        P.op("dve", lambda e: e.tensor_copy(out=X1[hi], in_=Bb[1][hi]), r=[rG], w=[rG])
        P.op("dve", lambda e: e.tensor_copy(out=X2[lo], in_=Bb[1][lo]), r=[rG], w=[rG])
        P.op("dve", lambda e: e.tensor_copy(out=X2[hi], in_=Bb[0][hi]), r=[rG], w=[rG])
        P.op("dve", lambda e: e.tensor_copy(out=Y1[lo], in_=cT[0][lo]), r=[rG], w=[rG])
        P.op("dve", lambda e: e.tensor_scalar(out=Y1[hi], in0=cT[1][hi], scalar1=-1.0, scalar2=None, op0=ALU.mult), r=[rG], w=[rG])
        P.op("dve", lambda e: e.tensor_copy(out=Y2[lo], in_=cT[1][lo]), r=[rG], w=[rG])
        P.op("dve", lambda e: e.tensor_copy(out=Y2[hi], in_=cT[0][hi]), r=[rG], w=[rG])
        for s in range(8):
            ki = 7 - s
            P.op("dve", lambda e, ki=ki: e.tensor_tensor(out=tmpA, in0=X2, in1=bc16(PBs[:, ki, :]), op=ALU.mult), r=[rG], w=[rG])
            P.op("dve", lambda e, ki=ki, s=s: e.tensor_tensor(out=BpT[:, :, s, :], in0=X1, in1=bc16(pw_re[:, ki, :]), op=ALU.mult), r=[rG], w=[rG])
            P.op("dve", lambda e, s=s: e.tensor_tensor(out=BpT[:, :, s, :], in0=BpT[:, :, s, :], in1=tmpA, op=ALU.add), r=[rG], w=[rG])
        for t in range(8):
            P.op("dve", lambda e, t=t: e.tensor_tensor(out=tmpA, in0=Y2, in1=bc16(pw_im[:, t, :]), op=ALU.mult), r=[rG], w=[rG])
            P.op("dve", lambda e, t=t: e.tensor_tensor(out=CpK[:, :, t, :], in0=Y1, in1=bc16(pw_re[:, t, :]), op=ALU.mult), r=[rG], w=[rG])
            P.op("dve", lambda e, t=t: e.tensor_tensor(out=CpK[:, :, t, :], in0=CpK[:, :, t, :], in1=tmpA, op=ALU.subtract), r=[rG], w=[rG])
        for t in range(8):
            P.op("dve", lambda e, t=t: e.tensor_tensor(out=tmpA, in0=Y2, in1=bc16(pw_im[:, t + 1, :]), op=ALU.mult), r=[rG], w=[rG])
            P.op("dve", lambda e, t=t: e.tensor_tensor(out=Cpw[:, :, t, :], in0=Y1, in1=bc16(pw_re[:, t + 1, :]), op=ALU.mult), r=[rG], w=[rW])
            P.op("dve", lambda e, t=t: e.tensor_tensor(out=Cpw[:, :, t, :], in0=Cpw[:, :, t, :], in1=tmpA, op=ALU.subtract), r=[rG, rW], w=[rW])
        rZ = RL(2)
        for i in range(2):
            P.op("pool", lambda e, i=i: e.memset(Zt[i][:, 0:240], 0.0), w=[rZ[i]])
        for g in range(32):
            b = 4 + g % 2
            P.op("pe", lambda e, g=g, b=b: e.transpose(psb(b, 128), BpT[:, g, :, :].rearrange("p s c -> p (s c)"), ident_f), r=[rG, rC], w=[RB[b]])
            P.op("act", lambda e, g=g, b=b: e.activation(out=Bpow[:, g, :], in_=psb(b, 128), func=AF.Identity), r=[RB[b]], w=[rW])
            z = Zt[g % 2]
            P.op("dve", lambda e, g=g, z=z: e.tensor_copy(out=z[:, 112:128], in_=X1[:, g, :]), r=[rG], w=[rZ[g % 2]])
            b2 = 6 + g % 2
            for s in range(8):
                P.op("pe", lambda e, g=g, s=s, z=z, b2=b2: e.matmul(psb(b2, 128 - 16 * s, off=16 * s), lhsT=z[:, (7 - s) * 16:(7 - s) * 16 + 128],
                                                                     rhs=CpK[:, g, 0:8 - s, :].rearrange("p t c -> p (t c)"), start=(s == 0), stop=(s == 7), skip_group_check=True),
                     r=[rZ[g % 2], rG], w=[RB[b2]])
            P.op("dve", lambda e, g=g, b2=b2: e.scalar_tensor_tensor(out=Toep[:, g, :], in0=ident_f, scalar=dcol[:, g:g + 1], in1=psb(b2, 128), op0=ALU.mult, op1=ALU.add),
                 r=[RB[b2], rG, rC], w=[rW])
        P.barrier()
        if stop == 2:
            return finish()

        m0 = 120.0
        Dk = A(m0, 18432, BF16).rearrange("p (g j m) -> p g j m", g=8, j=9)
        Ug = [A(m0 + 18 + i, 1024, BF16) for i in range(2)]
        Xs = [A(m0 + 20 + i, 1024, BF16) for i in range(4)]
        gw = [[A(m0 + 24 + 6 * i + 2 * k_, 2048) for k_ in range(3)] for i in range(2)]
        zg = [A(m0 + 36 + i, 1024, BF16) for i in range(2)]
        Tt = A(m0 + 38, 512)
        rDk, rUg, rX, rGw, rZg, rZtok, rTt = RL(8), RL(2), RL(4), RL(2), RL(2), RL(4), R()
        for gb in range(4):
            for gl in range(8):
                g = gb * 8 + gl
                for j in range(9):
                    ki = 8 + j
                    P.op("act", lambda e, g=g, ki=ki: e.activation(out=Tt[:, 0:128], in_=ident_f, func=AF.Identity, scale=pw_re[:, ki, g:g + 1]), r=[rW, rC], w=[rTt])
                    P.op("dve", lambda e, g=g, gl=gl, j=j, ki=ki: e.scalar_tensor_tensor(out=Dk[:, gl, j, :], in0=J_f, scalar=PW2[:, ki, g:g + 1], in1=Tt[:, 0:128], op0=ALU.mult, op1=ALU.add),
                         r=[rTt, rW, rC], w=[rDk[gl]])
            def grp(gl, gb=gb):
                g = gb * 8 + gl
                if cut < 1:
                    return
                ug = Ug[g % 2]
                pb = g % 2
                for nt in range(4):
                    P.op("pe", lambda e, g=g, nt=nt, pb=pb: e.transpose(psb(pb, 128, BF16, off=nt * 128), Un[nt][:, g, :, :].rearrange("p s c -> p (s c)"), ident_b), r=[rUn[nt], rC], w=[RB[pb]])
                P.op("act", lambda e, ug=ug, pb=pb: e.activation(out=ug, in_=psb(pb, 512, BF16), func=AF.Identity), r=[RB[pb]], w=[rUg[g % 2]])
                kb = 2 + g % 2
                xa = (g % 2) * 2
                P.op("pe", lambda e, g=g, ug=ug, kb=kb: e.matmul(psb(kb), lhsT=Bpow[:, g, :], rhs=ug, start=True, stop=True), r=[rW, rUg[g % 2]], w=[RB[kb]])
                P.op("dve", lambda e, kb=kb, xa=xa: e.tensor_copy(out=Xs[xa], in_=psb(kb)), r=[RB[kb]], w=[rX[xa]])
                yield
                cur = xa
                if cut < 2:
                    return
                for j in range(9 if cut >= 3 else 0):
                    sh = 1 << j
                    nxt = xa + (1 - (cur - xa))
                    P.op("pe", lambda e, cur=cur, kb=kb: e.matmul(psb(kb), lhsT=ident_b, rhs=Xs[cur], start=True, stop=False, skip_group_check=True), r=[rX[cur], rC], w=[RB[kb]])
                    P.op("pe", lambda e, cur=cur, kb=kb, sh=sh, gl=gl, j=j: e.matmul(psb(kb, 512 - sh, off=sh), lhsT=Dk[:, gl, j, :], rhs=Xs[cur][:, 0:512 - sh], start=False, stop=True, skip_group_check=True),
                         r=[rX[cur], rDk[gl]], w=[RB[kb]])
                    if j % 2 == 0:
                        P.op("act", lambda e, nxt=nxt, kb=kb: e.activation(out=Xs[nxt], in_=psb(kb), func=AF.Identity), r=[RB[kb]], w=[rX[nxt]])
                    else:
                        P.op("dve", lambda e, nxt=nxt, kb=kb: e.tensor_copy(out=Xs[nxt], in_=psb(kb)), r=[RB[kb]], w=[rX[nxt]])
                    cur = nxt
                    yield
                if cut < 4:
                    return
                P.op("pe", lambda e, g=g, ug=ug, pb=pb: e.matmul(psb(pb), lhsT=Toep[:, g, :], rhs=ug, start=True, stop=False, skip_group_check=True), r=[rW, rUg[g % 2]], w=[RB[pb]])
                P.op("pe", lambda e, g=g, cur=cur, pb=pb: e.matmul(psb(pb, 511, off=1), lhsT=Cpw[:, g, :, :].rearrange("p t c -> p (t c)"), rhs=Xs[cur][:, 0:511], start=False, stop=True, skip_group_check=True),
                     r=[rW, rX[cur]], w=[RB[pb]])
                yield
                if cut < 5:
                    return
                w0, w1_, w2_ = gw[g % 2]
                rg = rGw[g % 2]
                P.op("act", lambda e, w0=w0, pb=pb: e.activation(out=w0[:, 0:512], in_=psb(pb), func=AF.Square), r=[RB[pb]], w=[rg])
                P.op("dve", lambda e, w0=w0: e.tensor_scalar(out=w0[:, 0:512], in0=w0[:, 0:512], scalar1=0.044715, scalar2=1.0, op0=ALU.mult, op1=ALU.add), r=[rg], w=[rg])
                P.op("dve", lambda e, w0=w0, w1_=w1_, pb=pb: e.tensor_tensor(out=w1_[:, 0:512], in0=w0[:, 0:512], in1=psb(pb), op=ALU.mult), r=[rg, RB[pb]], w=[rg])
                P.op("act", lambda e, w1_=w1_: e.activation(out=w1_[:, 0:512], in_=w1_[:, 0:512], func=AF.Sigmoid, scale=1.5957691216), r=[rg], w=[rg])
                zz = zg[g % 2]
                P.op("dve", lambda e, w1_=w1_, zz=zz, pb=pb: e.tensor_tensor(out=zz, in0=w1_[:, 0:512], in1=psb(pb), op=ALU.mult), r=[rg, RB[pb]], w=[rZg[g % 2]])
                yield
                if cut < 6:
                    return
                tb = 4 + g % 2
                for nt in range(4):
                    P.op("pe", lambda e, zz=zz, nt=nt, tb=tb: e.transpose(psb(tb, 128, BF16, off=nt * 128), zz[:, nt * 128:(nt + 1) * 128], ident_b), r=[rZg[g % 2], rC], w=[RB[tb]])
                for nt in range(4):
                    src = psb(tb, 128, BF16, off=nt * 128).rearrange("p (t c) -> p t c", t=8)
                    eng = "act"
                    if eng == "act":
                        P.op("act", lambda e, src=src, nt=nt, g=g: e.activation(out=Ztok[nt][:, :, 16 * g:16 * g + 16], in_=src, func=AF.Identity), r=[RB[tb]], w=[rZtok[nt]])
                    else:
                        P.op("dve", lambda e, src=src, nt=nt, g=g: e.tensor_copy(out=Ztok[nt][:, :, 16 * g:16 * g + 16], in_=src), r=[RB[tb]], w=[rZtok[nt]])
            for gp in range(4):
                alive = [grp(2 * gp), grp(2 * gp + 1)]
                while alive:
                    for gn in list(alive):
                        try:
                            next(gn)
                        except StopIteration:
                            alive.remove(gn)
        P.barrier()
        if stop == 3:
            return finish()
        zT = [A(24 + 8 * cc, 8192, BF16) for cc in range(4)]
        rzT = RL(4)
        k_ = 0
        for nt in range(4):
            for cc in range(4):
                b = k_ % 4
                k_ += 1
                for t in range(8):
                    P.op("pe", lambda e, nt=nt, cc=cc, t=t, b=b: e.transpose(psb(b, 128, BF16, off=t * 128), Ztok[nt][:, t, cc * 128:(cc + 1) * 128], ident_b), r=[rZtok[nt], rC], w=[RB[b]])
                dst = zT[cc][:, nt * 1024:(nt + 1) * 1024].rearrange("p (n t) -> p t n", t=8)
                src = psb(b, 1024, BF16).rearrange("p (t n) -> p t n", t=8)
                P.op("act", lambda e, dst=dst, src=src: e.activation(out=dst, in_=src, func=AF.Identity), r=[RB[b]], w=[rzT[cc]])
        P.barrier()
        if stop == 4:
            return finish()
        soT = [A(56 + 8 * cc, 8192, BF16) for cc in range(4)]
        wg_st = A(120, 8192).rearrange("p (k n) -> p k n", k=4)
        Wg = A(128, 4096, BF16).rearrange("p (k n) -> p k n", k=4)
        sgw = [A(132 + i, 1024, BF16) for i in range(2)]
        rWg, rSg, rSo = R(), RL(2), RL(4)
        P.dma(lambda e: e.dma_start(out=wg_st, in_=wglu_d.rearrange("(k p) n -> p k n", p=128)), w=[rWg])
        P.op("pool", lambda e: e.tensor_copy(out=Wg, in_=wg_st), r=[rWg], w=[rWg])
        k_ = 0
        for sl in range(8):
            for co in range(4):
                b = k_ % 4
                sg_ = sgw[k_ % 2]
                rs_ = rSg[k_ % 2]
                k_ += 1
                for ci in range(4):
                    P.op("pe", lambda e, co=co, ci=ci, sl=sl, b=b: e.matmul(psb(b), lhsT=Wg[:, ci, co * 128:(co + 1) * 128], rhs=zT[ci][:, sl * 512:(sl + 1) * 512], start=(ci == 0), stop=(ci == 3)),
                         r=[rWg, rzT[ci]], w=[RB[b]])
                P.op("act", lambda e, co=co, b=b, sg_=sg_: e.activation(out=sg_, in_=psb(b), func=AF.Sigmoid, bias=bglu_c[:, co:co + 1]), r=[RB[b], rC], w=[rs_])
                P.op("dve", lambda e, co=co, sl=sl, sg_=sg_: e.tensor_tensor(out=soT[co][:, sl * 512:(sl + 1) * 512], in0=zT[co][:, sl * 512:(sl + 1) * 512], in1=sg_, op=ALU.mult),
                     r=[rs_, rzT[co]], w=[rSo[co]])
        rSSO = R()
        for cc in range(4):
            P.dma(lambda e, cc=cc: e.dma_start(out=sso_d[cc], in_=soT[cc]), r=[rSo[cc]], w=[rSSO])
        if dbg:
            dz = dout("dbg_so", [4, 128, S], BF16)
            for cc in range(4):
                P.dma(lambda e, cc=cc: e.dma_start(out=dz[cc], in_=soT[cc]), r=[rSo[cc]])
        P.barrier()
        if stop == 5:
            return finish()

        QT = [A(24 + 8 * h, 8192, BF16) for h in range(4)]
        KT = [A(56 + 8 * h, 8192, BF16) for h in range(4)]
        VA = A(88, 33024, BF16).rearrange("p (j h c) -> p j h c", j=32, h=4)
        Wq = A(122, 24576, BF16).rearrange("p (k n) -> p k n", k=8)
        wq_st = [A(146 + 6 * i, 6144) for i in range(2)]
        xr = [A(158 + 4 * i, 4096) for i in range(2)]
        xsr = [A(166 + 4 * i, 4096) for i in range(2)]
        h1g = A(174, 8192, BF16).rearrange("p (k n) -> p k n", k=8)
        stat1 = A(182, 64)
        rXr, rXs, rStat, rWq, rWst, rH = RL(2), RL(2), RL(4), R(), RL(2), RL(8)
        rQ, rK, rV = RL(4), RL(4), RL(32)
        for k in range(8):
            P.dma(lambda e, k=k: e.dma_start(out=wq_st[k % 2][:, 0:1536], in_=win_d[k * 128:(k + 1) * 128, 0:1536]), w=[rWst[k % 2]])
            P.op("pool", lambda e, k=k: e.tensor_copy(out=Wq[:, k, :], in_=wq_st[k % 2][:, 0:1536]), r=[rWst[k % 2]], w=[rWq])
        P.op("pool", lambda e: e.memset(VA[:, :, :, 128:129], 1.0), w=[rV])
        for tg in range(8):
            def ev(kc):
                dst = h1g[:, kc, :]
                if kc % 2 == 0:
                    P.op("act", lambda e, kc=kc, dst=dst: e.activation(out=dst, in_=psb(kc), func=AF.Identity, scale=A1c[:, kc:kc + 1], bias=B1c[:, kc:kc + 1]),
                         r=[RB[kc], rC], w=[rH[kc]])
                else:
                    P.op("dve", lambda e, kc=kc, dst=dst: e.tensor_scalar(out=dst, in0=psb(kc), scalar1=A1c[:, kc:kc + 1], scalar2=B1c[:, kc:kc + 1], op0=ALU.mult, op1=ALU.add),
                         r=[RB[kc], rC], w=[rH[kc]])
            norm_T(x_d, tg * 512, xr, xsr, A1c, B1c, ev, rXr, rXs, stat1, rStat)
            k_ = 0
            for h in range(4):
                for which in range(2):
                    b = k_ % 4
                    k_ += 1
                    col0 = which * 512 + h * 128
                    for kc in range(8):
                        P.op("pe", lambda e, kc=kc, b=b, col0=col0: e.matmul(psb(b), lhsT=Wq[:, kc, col0:col0 + 128], rhs=h1g[:, kc, :], start=(kc == 0), stop=(kc == 7)),
                             r=[rWq, rH[kc]], w=[RB[b]])
                    if which == 0:
                        P.op("act", lambda e, h=h, b=b, tg=tg: e.activation(out=QT[h][:, tg * 512:(tg + 1) * 512], in_=psb(b), func=AF.Identity, scale=0.125), r=[RB[b]], w=[rQ[h]])
                    else:
                        P.op("dve", lambda e, h=h, b=b, tg=tg: e.tensor_copy(out=KT[h][:, tg * 512:(tg + 1) * 512], in_=psb(b)), r=[RB[b]], w=[rK[h]])
            for i in range(4):
                j = tg * 4 + i
                b = 4 + i % 4
                for kc in range(8):
                    P.op("pe", lambda e, kc=kc, b=b, i=i: e.matmul(psb(b), lhsT=h1g[:, kc, i * 128:(i + 1) * 128], rhs=Wq[:, kc, 1024:1536], start=(kc == 0), stop=(kc == 7)),
                         r=[rWq, rH[kc]], w=[RB[b]])
                src = psb(b).rearrange("p (h c) -> p h c", h=4)
                if i % 2 == 0:
                    P.op("act", lambda e, j=j, src=src: e.activation(out=VA[:, j, :, 0:128], in_=src, func=AF.Identity), r=[RB[b]], w=[rV[j]])
                else:
                    P.op("dve", lambda e, j=j, src=src: e.tensor_copy(out=VA[:, j, :, 0:128], in_=src), r=[RB[b]], w=[rV[j]])
        P.barrier()
        if stop == 6:
            return finish()
        if dbg:
            dq = dout("dbg_qT", [128, S], BF16)
            dk_ = dout("dbg_kT", [128, S], BF16)
            dv = dout("dbg_v", [128, 516], BF16)
            P.dma(lambda e: e.dma_start(out=dq, in_=QT[1]))
            P.dma(lambda e: e.dma_start(out=dk_, in_=KT[1]))
            P.dma(lambda e: e.dma_start(out=dv, in_=VA[:, 3, :, :].rearrange("p h c -> p (h c)")))
            P.barrier()

        Wo = A(122, 16384, BF16).rearrange("p (k n) -> p k n", k=8)
        wo_st = [A(138 + 4 * i, 4096) for i in range(2)]
        PT = [[A(146 + 2 * m + i, 1024, BF16) for i in range(2)] for m in range(2)]
        ew = [[A(150 + i + 0.5 * k_, 512) for k_ in range(2)] for i in range(2)]
        atok = [A(154 + j, 1024, BF16) for j in range(4)]
        catT = A(158, 1024, BF16).rearrange("p (c n) -> p c n", c=4)
        ssl = [A(162 + 4 * i, 4096, BF16).rearrange("p (c n) -> p c n", c=4) for i in range(2)]
        xr = [A(170 + 4 * i, 4096) for i in range(2)]
        x1o = [A(178 + 4 * i, 4096) for i in range(2)]
        est = A(186, 64)
        rWo, rWos, rPT, rEw, rAt, rCat, rSsl, rXr, rX1, rEst = R(), RL(2), RL(2, 2), RL(2), RL(4), R(), RL(2), RL(2), RL(2), R()
        for k in range(8):
            P.dma(lambda e, k=k: e.dma_start(out=wo_st[k % 2], in_=wout_d[k * 128:(k + 1) * 128, :]), w=[rWos[k % 2]])
            P.op("pool", lambda e, k=k: e.tensor_copy(out=Wo[:, k, :], in_=wo_st[k % 2]), r=[rWos[k % 2]], w=[rWo])
        def oacc(m, j):
            idx = m * 4 + j
            return ps[:, 4 + idx // 3, (idx % 3) * 129:(idx % 3) * 129 + 129], 4 + idx // 3, (idx % 3 == 0)
        ecount = [0]
        steps = [(g, h, kb) for g in range(8) for h in range(4) for kb in range(4 * g + 4)]

        def emit_S(i):
            g, h, kb = steps[i]
            buf = i % 2
            if h == 0 and kb == 0:
                P.dma(lambda e, g=g: e.dma_start(out=ssl[g % 2], in_=sso_d[:, :, g * 512:(g + 1) * 512].rearrange("c p n -> p c n")), r=[rSSO], w=[rSsl[g % 2]])
            c0 = max(0, kb - 4 * g) * 128
            for m in range(2):
                sb = m * 2 + buf
                P.op("pe", lambda e, m=m, sb=sb, h=h, kb=kb, g=g, c0=c0: e.matmul(psb(sb, 512 - c0, off=c0), lhsT=KT[h][64 * m:64 * m + 64, kb * 128:(kb + 1) * 128],
                                                                                 rhs=QT[h][64 * m:64 * m + 64, g * 512 + c0:(g + 1) * 512], start=True, stop=True),
                     r=[rK[h], rQ[h]], w=[RB[sb]])

        def emit_PV(i):
            g, h, kb = steps[i]
            buf = i % 2
            j0 = max(0, kb - 4 * g)
            c0 = j0 * 128
            for m in range(2):
                sb = m * 2 + buf
                P.op("act", lambda e, m=m, sb=sb, buf=buf, c0=c0: e.activation(out=PT[m][buf][:, c0:512], in_=psb(sb, 512 - c0, off=c0), func=AF.Exp), r=[RB[sb]], w=[rPT[m][buf]])
                if kb >= 4 * g:
                    P.op("pool", lambda e, m=m, buf=buf, c0=c0: e.tensor_tensor(out=PT[m][buf][:, c0:c0 + 128], in0=PT[m][buf][:, c0:c0 + 128], in1=tri_b, op=ALU.mult),
                         r=[rPT[m][buf], rC], w=[rPT[m][buf]])
            for m in range(2):
                for j in range(j0, 4):
                    oap, ob, first = oacc(m, j)
                    P.op("pe", lambda e, m=m, j=j, buf=buf, oap=oap, kb=kb, h=h, first=first, g=g: e.matmul(oap, lhsT=PT[m][buf][:, j * 128:(j + 1) * 128], rhs=VA[:, kb, h, :],
                                                                                                      start=(kb == 0 and first), stop=(kb == 4 * g + j), skip_group_check=True),
                         r=[rPT[m][buf], rV[kb]], w=[RB[ob]])

        def epilogue(g, h):
            for j in range(4):
                o1, b1_, _ = oacc(0, j)
                o2, b2_, _ = oacc(1, j)
                w_a, w_b = ew[ecount[0] % 2]
                re_ = rEw[ecount[0] % 2]
                ecount[0] += 1
                P.op("dve", lambda e, o1=o1: e.reciprocal(out=est[:, 0:1], in_=o1[:, 128:129]), r=[RB[b1_]], w=[rEst])
                P.op("dve", lambda e, o2=o2: e.reciprocal(out=est[:, 1:2], in_=o2[:, 128:129]), r=[RB[b2_]], w=[rEst])
                P.op("dve", lambda e: e.tensor_tensor(out=est[:, 1:2], in0=est[:, 1:2], in1=neglam[:, 0:1], op=ALU.mult), r=[rEst, rC], w=[rEst])
                P.op("act", lambda e, o1=o1, w_a=w_a: e.activation(out=w_a[:, 0:128], in_=o1[:, 0:128], func=AF.Identity, scale=est[:, 0:1]), r=[RB[b1_], rEst], w=[re_])
                P.op("dve", lambda e, o2=o2, w_a=w_a: e.scalar_tensor_tensor(out=w_a[:, 0:128], in0=o2[:, 0:128], scalar=est[:, 1:2], in1=w_a[:, 0:128], op0=ALU.mult, op1=ALU.add),
                     r=[RB[b2_], rEst, re_], w=[re_])
                P.op("act", lambda e, w_a=w_a, w_b=w_b: e.activation(out=w_b[:, 0:128], in_=w_a[:, 0:128], func=AF.Square, accum_out=est[:, 2:3]), r=[re_], w=[re_, rEst])
                P.op("dve", lambda e: e.tensor_scalar(out=est[:, 3:4], in0=est[:, 2:3], scalar1=1.0 / 128, scalar2=EPS, op0=ALU.mult, op1=ALU.add), r=[rEst], w=[rEst])
                P.op("act", lambda e: e.activation(out=est[:, 4:5], in_=est[:, 3:4], func=AF.Sqrt), r=[rEst], w=[rEst])
                P.op("dve", lambda e: e.reciprocal(out=est[:, 5:6], in_=est[:, 4:5]), r=[rEst], w=[rEst])
                P.op("dve", lambda e, w_a=w_a, j=j, h=h: e.scalar_tensor_tensor(out=atok[j][:, h * 128:(h + 1) * 128], in0=w_a[:, 0:128], scalar=est[:, 5:6], in1=subg_bc[:, 0:128],
                                                                             op0=ALU.mult, op1=ALU.mult), r=[re_, rEst, rC], w=[rAt[j]])

        def outproj(g):
            for j in range(4):
                T = 4 * g + j
                for c in range(4):
                    P.op("pe", lambda e, j=j, c=c: e.transpose(psb(7, 128, BF16, off=c * 128), atok[j][:, c * 128:(c + 1) * 128], ident_b), r=[rAt[j], rC], w=[RB[7]])
                P.op("act", lambda e: e.activation(out=catT.rearrange("p c n -> p (c n)"), in_=psb(7, 512, BF16), func=AF.Identity), r=[RB[7]], w=[rCat])
                xt = xr[T % 2]
                P.dma(lambda e, xt=xt, T=T: e.dma_start(out=xt, in_=x_d[T * 128:(T + 1) * 128, :]), w=[rXr[T % 2]])
                xo = x1o[T % 2]
                for sl in range(2):
                    for c in range(8):
                        if c < 4:
                            lhsT = catT[:, c, :]
                            rr = [rCat, rWo]
                        else:
                            lhsT = ssl[g % 2][:, c - 4, j * 128:(j + 1) * 128]
                            rr = [rSsl[g % 2], rWo]
                        P.op("pe", lambda e, lhsT=lhsT, c=c, sl=sl: e.matmul(psb(7), lhsT=lhsT, rhs=Wo[:, c, sl * 512:(sl + 1) * 512], start=(c == 0), stop=(c == 7)), r=rr, w=[RB[7]])
                    P.op("dve", lambda e, xo=xo, sl=sl: e.tensor_tensor(out=xo[:, sl * 512:(sl + 1) * 512], in0=psb(7), in1=g1_bc[:, sl * 512:(sl + 1) * 512], op=ALU.mult), r=[RB[7], rC], w=[rX1[T % 2]])
                P.op("pool", lambda e, xo=xo, xt=xt: e.tensor_tensor(out=xo, in0=xo, in1=xt, op=ALU.add), r=[rX1[T % 2], rXr[T % 2]], w=[rX1[T % 2]])
                P.dma(lambda e, xo=xo, T=T: e.dma_start(out=x1s_d[T * 128:(T + 1) * 128, :], in_=xo), r=[rX1[T % 2]], w=[rSSO])
                if dbg and T == 5:
                    da = dout("dbg_atok", [128, 512], BF16)
                    P.dma(lambda e, j=j: e.dma_start(out=da, in_=atok[j]), r=[rAt[j]])
        emit_S(0)
        for i, (g, h, kb) in enumerate(steps):
            if i + 1 < len(steps):
                emit_S(i + 1)
            emit_PV(i)
            if kb == 4 * g + 3:
                epilogue(g, h)
                if h == 3:
                    outproj(g)
        P.barrier()
        if stop == 7:
            return finish()
        if dbg:
            dx1 = dout("dbg_x1", [256, D])
            P.dma(lambda e: e.dma_start(out=dx1, in_=x1s_d[512:768, :]))
            P.barrier()

        h2T = A(24, 16384, BF16).rearrange("p (k n) -> p k n", k=8)
        h2f = A(40, 16384).rearrange("p (k n) -> p k n", k=8)
        acc = A(56, 32768).rearrange("p (t n) -> p t n", t=8)
        aT = A(88, 16384, BF16).rearrange("p (k n) -> p k n", k=8)
        W2b = [A(104 + 16 * i, 16384, BF16).rearrange("p (k n) -> p k n", k=8) for i in range(2)]
        w2st = [A(136 + 4 * i, 4096) for i in range(2)]
        w1st = [A(144 + 8 * i, 8192).rearrange("p (a k n) -> p a k n", a=2, k=8) for i in range(2)]
        W1t = [A(160 + 4 * i, 4096, BF16).rearrange("p (a k n) -> p a k n", a=2, k=8) for i in range(2)]
        wk = [[A(168 + 6 * i + 2 * k_, 2048) for k_ in range(3)] for i in range(2)]
        gates = A(180, 1024).rearrange("p (t n) -> p t n", t=8)
        xr = [A(181 + 4 * i, 4096) for i in range(2)]
        xsr = [A(189 + 4 * i, 4096) for i in range(2)]
        lgw = A(197, 1024)
        stat1 = A(198, 64)
        gTb = A(198.5, 256, BF16)
        gates_b = A(198.75, 512, BF16).rearrange("p (t n) -> p t n", t=8)
        b2b = A(199.25, 2048, BF16)
        P.op("dve", lambda e: e.tensor_copy(out=b2b[0:32, :], in_=b2t[0:32, :]), r=[rC], w=[rC])
        rXr, rXs, rStat, rH2, rH2f, rAcc, raT, rW2b, rW2s, rW1s, rW1t, rWk, rGates, rLg, rgT = \
            RL(2), RL(2), RL(4), RL(8), RL(8), RL(8), RL(8), RL(2), RL(2), RL(2), RL(2), RL(2, 3), R(), R(), R()
        w1cnt = 0
        wkcnt = 0

        def load_w1(idx, ee, fc):
            stg = w1st[idx % 2]
            w1t = W1t[idx % 2]
            rs_, rt_ = rW1s[idx % 2], rW1t[idx % 2]
            for a in range(2):
                c0 = a * 1024 + fc * 128
                P.dma(lambda e, stg=stg, ee=ee, a=a, c0=c0: e.dma_start(out=stg[:, a, :, :], in_=w1_d[ee, :, c0:c0 + 128].rearrange("(k p) n -> p k n", p=128)), w=[rs_])
            P.op("act", lambda e, stg=stg, w1t=w1t: e.activation(out=w1t.rearrange("p a k n -> p (a k n)"), in_=stg.rearrange("p a k n -> p (a k n)"), func=AF.Identity), r=[rs_], w=[rt_])

        def w2_dma(ee, k):
            P.dma(lambda e, ee=ee, k=k: e.dma_start(out=w2st[k % 2], in_=w2_d[ee, k * 128:(k + 1) * 128, :]), w=[rW2s[k % 2]])

        def w2_cast(ee, k):
            P.op("act", lambda e, ee=ee, k=k: e.activation(out=W2b[ee % 2][:, k, :], in_=w2st[k % 2], func=AF.Identity), r=[rW2s[k % 2]], w=[rW2b[ee % 2]])
        if dbg:
            dlg = dout("dbg_gates", [128, 8, 32])
        for qt in range(nqt):
            for half in range(2):
                def ev(kc, half=half):
                    P.op("act", lambda e, kc=kc: e.activation(out=h2f[:, kc, :], in_=psb(kc), func=AF.Identity, scale=A2c[:, kc:kc + 1], bias=B2c[:, kc:kc + 1]), r=[RB[kc], rC], w=[rH2f[kc]])
                    P.op("dve", lambda e, kc=kc, half=half: e.tensor_copy(out=h2T[:, kc, half * 512:(half + 1) * 512], in_=h2f[:, kc, :]), r=[rH2f[kc]], w=[rH2[kc]])
                norm_T(x1s_d, qt * 1024 + half * 512, xr, xsr, A2c, B2c, ev, rXr, rXs, stat1, rStat)
                for i in range(4):
                    t = half * 4 + i
                    for kc in range(8):
                        P.op("pe", lambda e, kc=kc, i=i: e.matmul(psb(0, 32), lhsT=h2f[:, kc, i * 128:(i + 1) * 128], rhs=wr_t.rearrange("p (k n) -> p k n", k=8)[:, kc, :], start=(kc == 0), stop=(kc == 7)),
                             r=[rH2f[kc], rC], w=[RB[0]])
                    lg, ex, mk, t8 = lgw[:, 0:32], lgw[:, 32:64], lgw[:, 64:96], lgw[:, 96:104]
                    P.op("dve", lambda e, lg=lg: e.tensor_tensor(out=lg, in0=psb(0, 32), in1=brt_bc[:, 0:32], op=ALU.add), r=[RB[0], rC], w=[rLg])
                    P.op("dve", lambda e, lg=lg, t8=t8: e.max(out=t8, in_=lg), r=[rLg], w=[rLg])
                    P.op("dve", lambda e, lg=lg, mk=mk, t8=t8: e.tensor_scalar(out=mk, in0=lg, scalar1=t8[:, 3:4], scalar2=None, op0=ALU.is_ge), r=[rLg], w=[rLg])
                    P.op("dve", lambda e, t8=t8: e.tensor_scalar(out=lgw[:, 128:129], in0=t8[:, 0:1], scalar1=-1.0, scalar2=None, op0=ALU.mult), r=[rLg], w=[rLg])
                    P.op("act", lambda e, lg=lg, ex=ex: e.activation(out=ex, in_=lg, func=AF.Exp, bias=lgw[:, 128:129]), r=[rLg], w=[rLg])
                    P.op("dve", lambda e, ex=ex, mk=mk: e.scalar_tensor_tensor(out=ex, in0=ex, scalar=1.0, in1=mk, op0=ALU.mult, op1=ALU.mult, accum_out=lgw[:, 129:130]), r=[rLg], w=[rLg])
                    P.op("dve", lambda e: e.reciprocal(out=lgw[:, 130:131], in_=lgw[:, 129:130]), r=[rLg], w=[rLg])
                    P.op("dve", lambda e, ex=ex, t=t: e.tensor_scalar(out=gates[:, t, :], in0=ex, scalar1=lgw[:, 130:131], scalar2=None, op0=ALU.mult), r=[rLg], w=[rGates])
            if dbg and qt == 0:
                P.dma(lambda e: e.dma_start(out=dlg, in_=gates), r=[rGates])
            for t in range(8):
                P.op("pool", lambda e, t=t: e.memset(acc[:, t, :], 0.0), w=[rAcc[t]])
            for ex_ in range(nexp):
                w2b = W2b[ex_ % 2]
                if ex_ == 0:
                    for k in range(8):
                        w2_dma(0, k)
                        w2_cast(0, k)
                for fc in range(8):
                    if ex_ == 0 and fc == 0:
                        load_w1(w1cnt, 0, 0)
                    w1t = W1t[w1cnt % 2]
                    rt_ = rW1t[w1cnt % 2]
                    w1cnt += 1
                    if fc < 7:
                        load_w1(w1cnt, ex_, fc + 1)
                    elif ex_ + 1 < nexp:
                        load_w1(w1cnt, ex_ + 1, 0)
                    if ex_ + 1 < nexp:
                        if fc > 0:
                            w2_cast(ex_ + 1, fc - 1)
                        w2_dma(ex_ + 1, fc)
                    for th in range(2):
                        bG, bU = (wkcnt % 2) * 2, (wkcnt % 2) * 2 + 1
                        wa, wb, wc = wk[wkcnt % 2]
                        rk = rWk[wkcnt % 2]
                        wkcnt += 1
                        for a, bb in ((0, bG), (1, bU)):
                            for kc in range(8):
                                P.op("pe", lambda e, a=a, bb=bb, kc=kc, w1t=w1t, th=th: e.matmul(psb(bb), lhsT=w1t[:, a, kc, :], rhs=h2T[:, kc, th * 512:(th + 1) * 512], start=(kc == 0), stop=(kc == 7)),
                                     r=[rt_, rH2[kc]], w=[RB[bb]])
                        cg = ex_ * 16 + fc
                        cu = ex_ * 16 + 8 + fc
                        rka, rkb, rkc = rk
                        P.op("dve", lambda e, wa=wa, bG=bG, cg=cg: e.tensor_scalar(out=wa[:, 0:512], in0=psb(bG), scalar1=b1cols[:, cg:cg + 1], scalar2=7.0, op0=ALU.add, op1=ALU.min), r=[RB[bG], rC], w=[rka])
                        P.op("act", lambda e, wa=wa, wb=wb: e.activation(out=wb[:, 0:512], in_=wa[:, 0:512], func=AF.Sigmoid, scale=1.702), r=[rka], w=[rkb])
                        P.op("pool", lambda e, wa=wa, wb=wb: e.tensor_tensor(out=wb[:, 0:512], in0=wb[:, 0:512], in1=wa[:, 0:512], op=ALU.mult), r=[rka, rkb], w=[rkb])
                        P.op("dve", lambda e, wc=wc, bU=bU, cu=cu: e.tensor_scalar(out=wc[:, 0:512], in0=psb(bU), scalar1=b1p1[:, cu:cu + 1], scalar2=8.0, op0=ALU.add, op1=ALU.min), r=[RB[bU], rC], w=[rkc])
                        P.op("dve", lambda e, wc=wc, wb=wb, fc=fc, th=th: e.scalar_tensor_tensor(out=aT[:, fc, th * 512:(th + 1) * 512], in0=wc[:, 0:512], scalar=-6.0, in1=wb[:, 0:512], op0=ALU.max, op1=ALU.mult),
                             r=[rkb, rkc], w=[raT[fc]])
                if ex_ + 1 < nexp:
                    w2_cast(ex_ + 1, 7)
                for t in range(8):
                    for sl in range(2):
                        bY = 4 + (t * 2 + sl) % 4
                        for fc in range(8):
                            P.op("pe", lambda e, t=t, sl=sl, fc=fc, bY=bY, w2b=w2b: e.matmul(psb(bY), lhsT=aT[:, fc, t * 128:(t + 1) * 128], rhs=w2b[:, fc, sl * 512:(sl + 1) * 512], start=(fc == 0), stop=(fc == 7)),
                                 r=[raT[fc], rW2b[ex_ % 2]], w=[RB[bY]])
                        P.op("dve", lambda e, t=t, sl=sl, bY=bY, ex_=ex_: e.scalar_tensor_tensor(out=acc[:, t, sl * 512:(sl + 1) * 512], in0=psb(bY), scalar=gates[:, t, ex_:ex_ + 1], in1=acc[:, t, sl * 512:(sl + 1) * 512],
                                                                                             op0=ALU.mult, op1=ALU.add), r=[RB[bY], rGates, rAcc[t]], w=[rAcc[t]])
            for t in range(8):
                T = qt * 8 + t
                if t == 0:
                    P.op("dve", lambda e: e.tensor_copy(out=gates_b, in_=gates), r=[rGates], w=[rGates])
                P.op("pe", lambda e, t=t: e.transpose(ps[0:32, 0, :].bitcast(BF16)[:, 0:128], gates_b[:, t, :], ident_b), r=[rGates, rC], w=[RB[0]])
                P.op("act", lambda e: e.activation(out=gTb[0:32, 0:128], in_=ps[0:32, 0, :].bitcast(BF16)[:, 0:128], func=AF.Identity), r=[RB[0]], w=[rgT])
                xt = xr[t % 2]
                xo = xsr[t % 2]
                P.dma(lambda e, xt=xt, T=T: e.dma_start(out=xt, in_=x1s_d[T * 128:(T + 1) * 128, :]), w=[rXr[t % 2]])
                for sl in range(2):
                    bb = 1 + sl
                    P.op("pe", lambda e, sl=sl, bb=bb: e.matmul(psb(bb), lhsT=gTb[0:32, 0:128], rhs=b2b[0:32, sl * 512:(sl + 1) * 512], start=True, stop=True), r=[rgT, rC], w=[RB[bb]])
                    P.op("dve", lambda e, t=t, sl=sl, bb=bb: e.tensor_tensor(out=acc[:, t, sl * 512:(sl + 1) * 512], in0=acc[:, t, sl * 512:(sl + 1) * 512], in1=psb(bb), op=ALU.add), r=[RB[bb], rAcc[t]], w=[rAcc[t]])
                P.op("pool", lambda e, t=t: e.tensor_tensor(out=acc[:, t, :], in0=acc[:, t, :], in1=g2_bc, op=ALU.mult), r=[rAcc[t], rC], w=[rAcc[t]])
                P.op("dve", lambda e, t=t, xt=xt: e.tensor_tensor(out=xt, in0=acc[:, t, :], in1=xt, op=ALU.add), r=[rAcc[t], rXr[t % 2]], w=[rXr[t % 2]])
                P.op("act", lambda e, xt=xt, xo=xo: e.activation(out=xo, in_=xt, func=AF.Square, accum_out=stat1[:, 0:1]), r=[rXr[t % 2]], w=[rXs[t % 2], rStat[0]])
                P.op("dve", lambda e: e.tensor_scalar(out=stat1[:, 1:2], in0=stat1[:, 0:1], scalar1=1.0 / D, scalar2=EPS, op0=ALU.mult, op1=ALU.add), r=[rStat[0]], w=[rStat[0]])
                P.op("act", lambda e: e.activation(out=stat1[:, 2:3], in_=stat1[:, 1:2], func=AF.Sqrt), r=[rStat[0]], w=[rStat[0]])
                P.op("dve", lambda e: e.reciprocal(out=stat1[:, 3:4], in_=stat1[:, 2:3]), r=[rStat[0]], w=[rStat[0]])
                P.op("dve", lambda e, xt=xt, xo=xo: e.scalar_tensor_tensor(out=xo, in0=xt, scalar=stat1[:, 3:4], in1=fg_bc, op0=ALU.mult, op1=ALU.mult), r=[rXr[t % 2], rStat, rC], w=[rXs[t % 2]])
                P.dma(lambda e, xo=xo, T=T: e.dma_start(out=out_d[T * 128:(T + 1) * 128, :], in_=xo), r=[rXs[t % 2]], w=[rSSO])
            P.barrier()
        with nc.Block() as block:
            P.replay(block)
    return nc


_CACHE = {}


def _prep_inputs(inp, b):
    f = lambda a: np.ascontiguousarray(np.asarray(a, dtype=np.float32))
    m = {
        "x": f(inp["x"][b]), "c": f(inp["c"][b]),
        "w_ada": f(inp["w_ada"][0]), "b_ada": f(inp["b_ada"][0]), "norm1_g": f(inp["norm1_g"][0]),
        "w_in": f(inp["w_in"][0]),
        "lqk": f(np.stack([inp["lq1"][0], inp["lk1"][0], inp["lq2"][0], inp["lk2"][0]], 0)),
        "subln_g": f(inp["subln_g"][0]),
        "ssm_a_re": f(inp["ssm_a_re"][0]), "ssm_a_im": f(inp["ssm_a_im"][0]), "ssm_log_dt": f(inp["ssm_log_dt"][0]),
        "ssm_b_re": f(inp["ssm_b_re"][0]), "ssm_b_im": f(inp["ssm_b_im"][0]),
        "ssm_c_re": f(inp["ssm_c_re"][0]), "ssm_c_im": f(inp["ssm_c_im"][0]),
        "ssm_d": f(inp["ssm_d"][0]), "w_glu": f(inp["w_glu"][0]), "b_glu": f(inp["b_glu"][0]),
        "w_out": f(inp["w_out"][0]), "norm2_g": f(inp["norm2_g"][0]),
        "w_router": f(inp["w_router"][0]), "b_router": f(inp["b_router"][0]),
        "w1": f(inp["w1"][0]), "b1": f(inp["b1"][0]), "w2": f(inp["w2"][0]), "b2": f(inp["b2"][0]),
        "final_g": f(inp["final_g"]),
    }
    return m


def kernel(**inputs):
    if "nc" not in _CACHE:
        _CACHE["nc"] = build(False)
    nc = _CACHE["nc"]
    in_maps = [_prep_inputs(inputs, b) for b in range(8)]
    res = run_bass_kernel_spmd(nc, in_maps, core_ids=list(range(8)))
    out = np.stack([np.asarray(r["out"], dtype=np.float32) for r in res.results], 0)
    return out
```
